# Optimizing a Trainium2 kernel written in Bass

```python
import math
import jax, jax.numpy as jnp
from jax import lax
import numpy as np

D_MODEL = 2048
BATCH = 4
SEQ = 4096
DEPTH = 1

A_WIDTH = D_MODEL // 2
A_HEAD_DIM = 128
A_HEADS = A_WIDTH // A_HEAD_DIM
CHUNK = 128
B_WIDTH = D_MODEL - A_WIDTH
B_HEADS = 8
B_V_DIM = B_WIDTH // B_HEADS
B_QK_DIM = B_V_DIM // 2
B_QK_WIDTH = B_HEADS * 2 * B_QK_DIM
ROT_DIM = B_QK_DIM // 4
ROPE_THETA = 500000.0
Q_BLOCK = 128
IN_WIDTH = 2 * A_WIDTH + 2 * B_QK_WIDTH + B_WIDTH
MIX_WIDTH = A_WIDTH + B_WIDTH
N_GROUPS = 4
EXPERTS_PER_GROUP = 8
N_EXPERTS = N_GROUPS * EXPERTS_PER_GROUP
TOP_K = 2
D_EXPERT = D_MODEL // 2
EXPERT_BLOCK = 256
EPS = 1e-6

kernel_name = "hybrid_gmlp_diffattn_hiermoe_encoder"


def rms_norm(x, g, eps=EPS):
    xf = x.astype(jnp.float32)
    y = xf * lax.rsqrt(jnp.mean(xf * xf, axis=-1, keepdims=True) + eps)
    return (y * g.astype(jnp.float32)).astype(x.dtype)


def layer_norm(x, g, b, eps=EPS):
    xf = x.astype(jnp.float32)
    mu = jnp.mean(xf, axis=-1, keepdims=True)
    xc = xf - mu
    y = xc * lax.rsqrt(jnp.mean(xc * xc, axis=-1, keepdims=True) + eps)
    return (y * g.astype(jnp.float32) + b.astype(jnp.float32)).astype(x.dtype)


def rotary_tables(seq_len):
    pos = jnp.arange(seq_len, dtype=jnp.float32)
    inv_freq = 1.0 / (jnp.float32(ROPE_THETA) ** (jnp.arange(0, ROT_DIM, 2, dtype=jnp.float32) / ROT_DIM))
    ang = pos[:, None] * inv_freq[None, :]
    return jnp.cos(ang), jnp.sin(ang)


def partial_rotary(x, cos, sin):
    half = ROT_DIM // 2
    c = cos[None, :, None, None, :].astype(x.dtype)
    s = sin[None, :, None, None, :].astype(x.dtype)
    x1 = x[..., :half]
    x2 = x[..., half:ROT_DIM]
    return jnp.concatenate([x1 * c - x2 * s, x2 * c + x1 * s, x[..., ROT_DIM:]], axis=-1)


def gmlp_spatial_gating(zu, zv, ln_g, ln_b, w_s, b_s):
    bsz, seq, _ = zu.shape
    u = jax.nn.gelu(zu).reshape(bsz, seq, A_HEADS, A_HEAD_DIM)
    v = layer_norm(jax.nn.gelu(zv).reshape(bsz, seq, A_HEADS, A_HEAD_DIM), ln_g, ln_b)
    vc = v.reshape(bsz, seq // CHUNK, CHUNK, A_HEADS, A_HEAD_DIM)
    s = jnp.einsum('hij,bcjhd->bcihd', w_s, vc) + jnp.transpose(b_s)[:, :, None]
    return (u * s.reshape(bsz, seq, A_HEADS, A_HEAD_DIM)).reshape(bsz, seq, A_WIDTH)


def diff_attention_core(q, k, v, lam):
    bsz, seq, nh, _, dk = q.shape
    nqb = seq // Q_BLOCK
    qb = jnp.moveaxis(q.reshape(bsz, nqb, Q_BLOCK, nh, 2, dk), 1, 0)
    scale = 1.0 / math.sqrt(dk)

    def one_block(q_blk):
        s = jnp.einsum('bqhcd,bkhcd->bhcqk', q_blk, k).astype(jnp.float32) * scale
        p = jax.nn.softmax(s, axis=-1)
        a = p[:, :, 0] - lam * p[:, :, 1]
        return jnp.einsum('bhqk,bkhe->bqhe', a.astype(v.dtype), v)

    o = lax.map(one_block, qb)
    return jnp.moveaxis(o, 0, 1).reshape(bsz, seq, nh, -1)


def hierarchical_moe(h, w_group, b_group, w_router, b_router, w_gate, w_up, w_down):
    bsz, seq, d = h.shape
    T = bsz * seq
    hf = h.reshape(T, d)
    g_prob = jax.nn.softmax((hf @ w_group).astype(jnp.float32) + b_group.astype(jnp.float32), axis=-1)
    g_w, g_idx = lax.top_k(g_prob, 1)
    e_logits = ((hf @ w_router).astype(jnp.float32) + b_router.astype(jnp.float32)).reshape(T, N_GROUPS, EXPERTS_PER_GROUP)
    sel = jnp.take_along_axis(e_logits, g_idx[:, :, None], axis=1)[:, 0]
    e_w, e_loc = lax.top_k(jax.nn.softmax(sel, axis=-1), TOP_K)
    e_w = e_w / jnp.sum(e_w, axis=-1, keepdims=True)
    combine = g_w * e_w
    expert_id = g_idx * EXPERTS_PER_GROUP + e_loc

    A = T * TOP_K
    flat_e = expert_id.reshape(A)
    flat_t = jnp.repeat(jnp.arange(T, dtype=jnp.int32), TOP_K)
    flat_w = combine.reshape(A)
    order = jnp.argsort(flat_e)
    e_s = flat_e[order]
    counts = jnp.bincount(flat_e, length=N_EXPERTS)
    starts = jnp.cumsum(counts) - counts
    padded = (counts + EXPERT_BLOCK - 1) // EXPERT_BLOCK * EXPERT_BLOCK
    pend = jnp.cumsum(padded)
    pstart = pend - padded
    dest = pstart[e_s] + (jnp.arange(A) - starts[e_s])
    n_blocks = -(-A // EXPERT_BLOCK) + N_EXPERTS
    R = n_blocks * EXPERT_BLOCK
    row_tok = jnp.zeros((R,), jnp.int32).at[dest].set(flat_t[order])
    row_w = jnp.zeros((R,), jnp.float32).at[dest].set(flat_w[order])
    block_e = jnp.minimum(jnp.searchsorted(pend, jnp.arange(n_blocks) * EXPERT_BLOCK, side='right'), N_EXPERTS - 1)

    def expert_block(args):
        tok, e = args
        xb = hf[tok]
        return (jax.nn.silu(xb @ w_gate[e]) * (xb @ w_up[e])) @ w_down[e]

    y = lax.map(expert_block, (row_tok.reshape(n_blocks, EXPERT_BLOCK), block_e))
    y = y.reshape(R, d) * row_w[:, None].astype(y.dtype)
    out = jnp.zeros((T, d), h.dtype).at[row_tok].add(y)
    return out.reshape(bsz, seq, d)


def setup_inputs(seed: int = 0) -> dict:
    key = jax.random.key(seed)
    ks = jax.random.split(key, 24)
    f32 = jnp.float32
    L = DEPTH
    nrm = lambda k, shape, scale: jax.random.normal(k, shape, f32) * scale
    return {
        "x": jax.random.normal(ks[0], (BATCH, SEQ, D_MODEL), f32),
        "attn_norm_g": 1.0 + nrm(ks[1], (L, D_MODEL), 0.02),
        "w_in": nrm(ks[2], (L, D_MODEL, IN_WIDTH), D_MODEL ** -0.5),
        "gmlp_ln_g": 1.0 + nrm(ks[3], (L, A_HEADS, A_HEAD_DIM), 0.02),
        "gmlp_ln_b": nrm(ks[4], (L, A_HEADS, A_HEAD_DIM), 0.02),
        "gmlp_ws": nrm(ks[5], (L, A_HEADS, CHUNK, CHUNK), CHUNK ** -0.5),
        "gmlp_bs": 1.0 + nrm(ks[6], (L, A_HEADS, CHUNK), 0.02),
        "q_norm_g": 1.0 + nrm(ks[7], (L, B_QK_DIM), 0.02),
        "k_norm_g": 1.0 + nrm(ks[8], (L, B_QK_DIM), 0.02),
        "lambda_q1": nrm(ks[9], (L, B_QK_DIM), 0.1),
        "lambda_k1": nrm(ks[10], (L, B_QK_DIM), 0.1),
        "lambda_q2": nrm(ks[11], (L, B_QK_DIM), 0.1),
        "lambda_k2": nrm(ks[12], (L, B_QK_DIM), 0.1),
        "subln_g": 1.0 + nrm(ks[13], (L, B_V_DIM), 0.02),
        "w_out": nrm(ks[14], (L, MIX_WIDTH, D_MODEL), MIX_WIDTH ** -0.5),
        "ffn_norm_g": 1.0 + nrm(ks[15], (L, D_MODEL), 0.02),
        "w_group": nrm(ks[16], (L, D_MODEL, N_GROUPS), D_MODEL ** -0.5),
        "b_group": nrm(ks[17], (L, N_GROUPS), 0.01),
        "w_router": nrm(ks[18], (L, D_MODEL, N_EXPERTS), D_MODEL ** -0.5),
        "b_router": nrm(ks[19], (L, N_EXPERTS), 0.01),
        "w_gate": nrm(ks[20], (L, N_EXPERTS, D_MODEL, D_EXPERT), D_MODEL ** -0.5),
        "w_up": nrm(ks[21], (L, N_EXPERTS, D_MODEL, D_EXPERT), D_MODEL ** -0.5),
        "w_down": nrm(ks[22], (L, N_EXPERTS, D_EXPERT, D_MODEL), D_EXPERT ** -0.5),
    }


def reference(x, attn_norm_g, w_in, gmlp_ln_g, gmlp_ln_b, gmlp_ws, gmlp_bs, q_norm_g, k_norm_g,
              lambda_q1, lambda_k1, lambda_q2, lambda_k2, subln_g, w_out, ffn_norm_g,
              w_group, b_group, w_router, b_router, w_gate, w_up, w_down):
    bsz, seq, _ = x.shape
    cos, sin = rotary_tables(seq)
    splits = [A_WIDTH, 2 * A_WIDTH, 2 * A_WIDTH + B_QK_WIDTH, 2 * A_WIDTH + 2 * B_QK_WIDTH]
    for l in range(DEPTH):
        lambda_init = 0.8 - 0.6 * math.exp(-0.3 * l)
        h = rms_norm(x, attn_norm_g[l])
        z = h @ w_in[l]
        zu, zv, zq, zk, zvb = jnp.split(z, splits, axis=-1)
        out_a = gmlp_spatial_gating(zu, zv, gmlp_ln_g[l], gmlp_ln_b[l], gmlp_ws[l], gmlp_bs[l])
        q = rms_norm(zq.reshape(bsz, seq, B_HEADS, 2, B_QK_DIM), q_norm_g[l])
        k = rms_norm(zk.reshape(bsz, seq, B_HEADS, 2, B_QK_DIM), k_norm_g[l])
        q = partial_rotary(q, cos, sin)
        k = partial_rotary(k, cos, sin)
        v = zvb.reshape(bsz, seq, B_HEADS, B_V_DIM)
        lam = (jnp.exp(jnp.sum(lambda_q1[l].astype(jnp.float32) * lambda_k1[l].astype(jnp.float32)))
               - jnp.exp(jnp.sum(lambda_q2[l].astype(jnp.float32) * lambda_k2[l].astype(jnp.float32)))
               + lambda_init)
        o = diff_attention_core(q, k, v, lam)
        out_b = (rms_norm(o, subln_g[l]) * (1.0 - lambda_init)).reshape(bsz, seq, B_WIDTH)
        x = x + jnp.concatenate([out_a, out_b], axis=-1) @ w_out[l]
        hm = rms_norm(x, ffn_norm_g[l])
        x = x + hierarchical_moe(hm, w_group[l], b_group[l], w_router[l], b_router[l],
                                 w_gate[l], w_up[l], w_down[l])
    return x
```

```python
import contextlib
import math
import numpy as np
import concourse.bass as bass
import concourse.mybir as mybir
from concourse.bass_utils import run_bass_kernel_spmd

F32 = mybir.dt.float32
BF16 = mybir.dt.bfloat16
I32 = mybir.dt.int32
U8 = mybir.dt.uint8
ALU = mybir.AluOpType
AF = mybir.ActivationFunctionType
AX = mybir.AxisListType

D = 2048
SEQ = 4096
NT_OWN = 16
NT_ALL = 32
CAP = 384
NE = 32
R_ROWS = NE * CAP
EPS = 1e-6
LAMBDA_INIT = 0.8 - 0.6 * math.exp(0.0)


class Prog:
    ENG = ('pe', 'dve', 'act', 'pool', 'sp')
    EPOCH = 8000
    NDMA = 8

    def __init__(self, nc, es):
        self.nc = nc
        self.es = es
        self.ops = {e: [] for e in self.ENG}
        self.lastw = {}
        self.readers = {}
        self.dma_n = {q: 0 for q in self.ENG}
        self.dma_last = {}

    def _deps(self, reads, writes):
        deps = {}

        def add(key, val):
            if deps.get(key, -1) < val:
                deps[key] = val
        for r in reads:
            if r in self.lastw:
                add(*self.lastw[r])
        for w in writes:
            if w in self.lastw:
                add(*self.lastw[w])
            for k, v in self.readers.get(w, {}).items():
                add(k, v)
        return deps

    def _update(self, tok, reads, writes):
        k, v = tok
        for r in reads:
            d = self.readers.setdefault(r, {})
            if d.get(k, -1) < v:
                d[k] = v
        for w in writes:
            self.lastw[w] = tok
            self.readers[w] = {}

    def op(self, eng, fn, reads=(), writes=()):
        idx = len(self.ops[eng])
        tok = (('op', eng), idx)
        deps = self._deps(reads, writes)
        if eng == 'pe':
            deps.pop(('op', 'pe'), None)
        self.ops[eng].append(dict(fn=fn, deps=deps, kind='op', flagged=False))
        self._update(tok, reads, writes)
        return tok

    def dma(self, q, fn, reads=(), writes=()):
        n = self.dma_n[q]
        self.dma_n[q] += 1
        slot = n % self.NDMA
        val = 16 * (n // self.NDMA + 1)
        tok = (('dma', q, slot), val)
        deps = self._deps(reads, writes)
        prev = self.dma_last.get((q, slot))
        if prev is not None and deps.get(prev[0], -1) < prev[1]:
            deps[prev[0]] = prev[1]
        self.dma_last[(q, slot)] = tok
        self.ops[q].append(dict(fn=fn, deps=deps, kind='dma', slot=slot))
        self._update(tok, reads, writes)
        return tok

    def _all_deps(self):
        deps = {}
        for (q, slot), tok in self.dma_last.items():
            deps[tok[0]] = tok[1]
        for e in self.ENG:
            if e == 'sp':
                continue
            for i in range(len(self.ops[e]) - 1, -1, -1):
                ent = self.ops[e][i]
                if ent['fn'] is not None and ent['kind'] == 'op':
                    deps[('op', e)] = i
                    break
        return deps

    def barrier(self):
        idx = len(self.ops['sp'])
        self.ops['sp'].append(dict(fn=lambda e: e.nop(), deps=self._all_deps(), kind='op', flagged=True))
        for e in self.ENG:
            if e != 'sp':
                self.ops[e].append(dict(fn=None, deps={('op', 'sp'): idx}, kind='op', flagged=False))
        self.lastw = {}
        self.readers = {}

    def finish(self):
        self.ops['sp'].append(dict(fn=None, deps=self._all_deps(), kind='op', flagged=False))

    def emit(self):
        nc, es = self.nc, self.es
        for e in self.ENG:
            for ent in self.ops[e]:
                for key, val in ent['deps'].items():
                    if key[0] == 'op':
                        self.ops[key[1]][val]['flagged'] = True
        self.cnt = {}
        self.esems = {}
        for e in self.ENG:
            c = 0
            arr = []
            for ent in self.ops[e]:
                if ent['kind'] == 'op' and ent['flagged'] and ent['fn'] is not None:
                    c += 1
                arr.append(c)
            self.cnt[e] = arr
            nep = (c + self.EPOCH - 1) // self.EPOCH
            self.esems[e] = [es.enter_context(nc.semaphore(f"s_{e}_{i}")) for i in range(max(nep, 1))]
        self.dsems = {}
        for q in self.ENG:
            if self.dma_n[q]:
                self.dsems[q] = [es.enter_context(nc.semaphore(f"d_{q}_{i}")) for i in range(self.NDMA)]
        block = es.enter_context(nc.Block())
        block.tensor(lambda e: self._replay('pe', e))
        block.vector(lambda e: self._replay('dve', e))
        block.scalar(lambda e: self._replay('act', e))
        block.gpsimd(lambda e: self._replay('pool', e))
        block.sync(lambda e: self._replay('sp', e))

    def _replay(self, E, eng):
        waited = {}
        for i, ent in enumerate(self.ops[E]):
            for key, val in ent['deps'].items():
                if key[0] == 'op':
                    c = self.cnt[key[1]][val]
                    if c == 0 or waited.get(key, 0) >= c:
                        continue
                    waited[key] = c
                    eng.wait_ge(self.esems[key[1]][(c - 1) // self.EPOCH], (c - 1) % self.EPOCH + 1)
                else:
                    if waited.get(key, 0) >= val:
                        continue
                    waited[key] = val
                    eng.wait_ge(self.dsems[key[1]][key[2]], val)
            if ent['fn'] is None:
                continue
            ins = ent['fn'](eng)
            if ent['kind'] == 'dma':
                ins.then_inc(self.dsems[E][ent['slot']], 16)
            elif ent['flagged']:
                c = self.cnt[E][i]
                ins.then_inc(self.esems[E][(c - 1) // self.EPOCH], 1)


class Arena:
    def __init__(self, nc, es, nbytes):
        self.t = es.enter_context(nc.sbuf_tensor("arena", [128, nbytes], U8))
        self.nbytes = nbytes
        self.off = 0
        self.base = 0
        self.n = 0

    def tile(self, shape, dt, name=None):
        sz = {F32: 4, BF16: 2, I32: 4}[dt]
        n = int(np.prod(shape[1:])) * sz
        n = (n + 63) // 64 * 64
        assert self.off + n <= self.nbytes, (self.off, n, self.nbytes, name)
        v = self.t[:, self.off:self.off + n]
        if n != int(np.prod(shape[1:])) * sz:
            v = self.t[:, self.off:self.off + int(np.prod(shape[1:])) * sz]
        v = v.bitcast(dt)
        if len(shape) == 3:
            v = v.rearrange("p (a b) -> p a b", a=shape[1])
        elif len(shape) == 4:
            v = v.rearrange("p (a b c) -> p a b c", a=shape[1], b=shape[2])
        self.off += n
        self.n += 1
        return v, (name or "t") + f"#{self.n}"

    def mark(self):
        self.base = self.off

    def reset(self):
        self.off = self.base


def build_program(debug_outs=()):
    nc = bass.Bass("TRN2", target_bir_lowering=False)
    es = contextlib.ExitStack()

    def din(name, shape, dt=F32):
        return nc.dram_tensor(name, shape, dt, kind="ExternalInput").ap()

    def dscr(name, shape, dt):
        kind = "ExternalOutput" if name in debug_outs else "Internal"
        return nc.dram_tensor(name, shape, dt, kind=kind).ap()

    x_d = din("x", [SEQ, D])
    cos_d = din("cos", [128, NT_ALL, 8])
    sin_d = din("sin", [128, NT_ALL, 8])
    g_attn_d = din("g_attn", [1, D])
    w_in_d = din("w_in", [D, 5120])
    lng_d = din("ln_g", [1, 1024])
    lnb_d = din("ln_b", [1, 1024])
    wsT_d = din("wsT", [128, 8, 128])
    bsT_d = din("bsT", [128, 8])
    qg_d = din("qg", [1, 512])
    kg_d = din("kg", [1, 512])
    lam_d = din("lamv", [1, 256])
    sub_d = din("subg", [1, 128])
    w_out_d = din("w_out", [D, D])
    g_ffn_d = din("g_ffn", [1, D])
    wr_d = din("wr", [D, 36])
    br_d = din("br", [1, 36])
    wg_d = din("w_gate", [NE, D, 1024])
    wu_d = din("w_up", [NE, D, 1024])
    wd_d = din("w_down", [NE, 1024, D])
    out_d = nc.dram_tensor("out", [NT_OWN * 128, D], F32, kind="ExternalOutput").ap()

    hT_s = dscr("hT_s", [NT_ALL, 128, 16, 128], BF16)
    qT_s = dscr("qT_s", [8, 128, 2048], BF16)
    kT_s = dscr("kT_s", [8, 128, 4096], BF16)
    v_s = dscr("v_s", [8, 128, NT_ALL, 128], BF16)
    mixT_s = dscr("mixT_s", [16, 128, 2048], BF16)
    x1_s = dscr("x1_s", [NT_OWN, 128, D], F32)
    xs_s = dscr("xs_s", [R_ROWS + 128, D], BF16)
    y_s = dscr("y_s", [R_ROWS + 128, D], F32)

    with es:
        P = Prog(nc, es)
        A = Arena(nc, es, 204 * 1024)
        psum_all = es.enter_context(nc.psum_tensor("psum_all", [128, 4096], F32))
        banks = [psum_all[:, i * 512:(i + 1) * 512] for i in range(8)]

        def OP(eng, meth, reads=(), writes=(), **kw):
            P.op(eng, lambda e: getattr(e, meth)(**kw), reads, writes)

        def DMA(q, out, in_, reads=(), writes=()):
            P.dma(q, lambda e: e.dma_start(out=out, in_=in_), reads, writes)

        def bc(ap, shape):
            return ap.to_broadcast(shape)

        ident, n_ident = A.tile([128, 128], BF16, "ident")
        identf, n_identf = A.tile([128, 128], F32, "identf")
        tri, n_tri = A.tile([128, 128], BF16, "tri")
        ones, n_ones = A.tile([128, 128], BF16, "ones")
        neghalf, n_nh = A.tile([128, 32], F32, "neghalf")
        ebase, n_eb = A.tile([128, 32], F32, "ebase")
        trash, n_trash = A.tile([128, 1], F32, "trash")
        neg_lam, n_nl = A.tile([128, 1], F32, "neg_lam")
        base_bc, n_base = A.tile([128, 32], F32, "base_bc")
        idx_all, n_idx = A.tile([128, NT_OWN, 2], I32, "idx_all")
        cw_all, n_cw = A.tile([128, NT_OWN, 2], F32, "cw_all")
        lamt, n_lamt = A.tile([128, 256], F32, "lamt")
        lamp, n_lamp = A.tile([128, 128], F32, "lamp")
        lams, n_lams = A.tile([128, 2], F32, "lams")
        A.mark()

        OP('pool', 'iota', writes=[n_identf], out=identf, pattern=[[1, 128]], base=0, channel_multiplier=-1,
           allow_small_or_imprecise_dtypes=True)
        OP('dve', 'tensor_scalar', reads=[n_identf], writes=[n_ident], out=ident, in0=identf, scalar1=0.0, scalar2=None,
           op0=ALU.is_equal)
        OP('dve', 'tensor_scalar', reads=[n_identf], writes=[n_tri], out=tri, in0=identf, scalar1=0.0, scalar2=None,
           op0=ALU.is_gt)
        OP('dve', 'tensor_scalar', reads=[n_identf, n_ident, n_tri], writes=[n_identf], out=identf, in0=identf, scalar1=0.0,
           scalar2=None, op0=ALU.is_equal)
        OP('dve', 'memset', writes=[n_ones], ap=ones, constant=1.0)
        OP('dve', 'memset', writes=[n_nh], ap=neghalf, constant=-0.5)
        OP('dve', 'memset', writes=[n_base], ap=base_bc, constant=0.0)
        OP('pool', 'iota', writes=[n_eb], out=ebase, pattern=[[CAP, 32]], base=0, channel_multiplier=0,
           allow_small_or_imprecise_dtypes=True)
        OP('pool', 'iota', writes=[n_trash], out=trash, pattern=[[1, 1]], base=R_ROWS, channel_multiplier=1,
           allow_small_or_imprecise_dtypes=True)
        DMA('sp', lamt, bc(lam_d, [128, 256]), writes=[n_lamt])
        OP('dve', 'tensor_tensor', reads=[n_lamt], writes=[n_lamp], out=lamp, in0=lamt[:, 0:128], in1=lamt[:, 128:256],
           op=ALU.mult)
        OP('dve', 'tensor_reduce', reads=[n_lamp], writes=[n_lams], out=lams, in_=lamp.rearrange("p (a b) -> p a b", a=2),
           axis=AX.X, op=ALU.add)
        OP('act', 'activation', reads=[n_lams], writes=[n_lams], out=lams, in_=lams, func=AF.Exp)
        OP('dve', 'tensor_tensor', reads=[n_lams], writes=[n_nl], out=neg_lam, in0=lams[:, 1:2], in1=lams[:, 0:1],
           op=ALU.subtract)
        OP('dve', 'tensor_scalar', reads=[n_nl], writes=[n_nl], out=neg_lam, in0=neg_lam, scalar1=-LAMBDA_INIT, scalar2=None,
           op0=ALU.add)

        def pair_view(i):
            return psum_all[:, i * 512:(i + 2) * 512].rearrange("p (a b) -> p a b", a=2)

        def bank_bf16(i):
            return banks[i][:, :].bitcast(BF16)

        g_attn, n_ga = A.tile([128, D], F32, "g_attn")
        DMA('sp', g_attn, bc(g_attn_d, [128, D]), writes=[n_ga])
        xt = [A.tile([128, D], F32, f"xt{i}") for i in range(2)]
        junk, n_junk = A.tile([128, D], BF16, "junk")
        hb = [A.tile([128, D], BF16, f"hb{i}") for i in range(2)]
        hTt = [A.tile([128, D], BF16, f"hTt{i}") for i in range(2)]
        st = [A.tile([128, 2], F32, f"st{i}") for i in range(2)]
        for t in range(NT_ALL):
            b = t % 2
            (x_t, n_x), (h_t, n_h), (hT_t, n_hT), (s_t, n_s) = xt[b], hb[b], hTt[b], st[b]
            DMA('sp', x_t, x_d[t * 128:(t + 1) * 128, :], writes=[n_x])
            OP('act', 'activation', reads=[n_x], writes=[n_junk, n_s], out=junk, in_=x_t, func=AF.Square,
               accum_out=s_t[:, 0:1])
            OP('act', 'activation', reads=[n_s], writes=[n_s], out=s_t[:, 1:2], in_=s_t[:, 0:1], func=AF.Sqrt,
               scale=1.0 / D, bias=EPS)
            OP('dve', 'reciprocal', reads=[n_s], writes=[n_s], out=s_t[:, 1:2], in_=s_t[:, 1:2])
            OP('dve', 'scalar_tensor_tensor', reads=[n_x, n_s, n_ga], writes=[n_h], out=h_t, in0=x_t, scalar=s_t[:, 1:2],
               in1=g_attn, op0=ALU.mult, op1=ALU.mult)
            for kc in range(16):
                bk = 6 + kc // 8
                OP('pe', 'transpose', reads=[n_h, n_ident], writes=[f"bank{bk}"],
                   out=bank_bf16(bk)[:, (kc % 8) * 128:(kc % 8 + 1) * 128], in_=h_t[:, kc * 128:(kc + 1) * 128],
                   identity=ident)
            OP('act', 'copy', reads=["bank6"], writes=[n_hT], out=hT_t[:, 0:1024], in_=bank_bf16(6))
            OP('dve', 'tensor_copy', reads=["bank7"], writes=[n_hT], out=hT_t[:, 1024:2048], in_=bank_bf16(7))
            DMA('pool', hT_s[t], hT_t.rearrange("p (a b) -> p a b", a=16), reads=[n_hT], writes=["hT_s"])
        P.barrier()
        A.reset()

        lng, n_lng = A.tile([128, 1024], F32, "lng")
        lnb, n_lnb = A.tile([128, 1024], F32, "lnb")
        qg, n_qg = A.tile([128, 512], F32, "qg")
        kg, n_kg = A.tile([128, 512], F32, "kg")
        wsT, n_wsT = A.tile([128, 8, 128], BF16, "wsT")
        bsT, n_bsT = A.tile([128, 8], F32, "bsT")
        cosT, n_cos = A.tile([128, NT_ALL, 8], F32, "cosT")
        sinT, n_sin = A.tile([128, NT_ALL, 8], F32, "sinT")
        DMA('sp', lng, bc(lng_d, [128, 1024]), writes=[n_lng])
        DMA('sp', lnb, bc(lnb_d, [128, 1024]), writes=[n_lnb])
        DMA('sp', qg, bc(qg_d, [128, 512]), writes=[n_qg])
        DMA('sp', kg, bc(kg_d, [128, 512]), writes=[n_kg])
        DMA('pool', wsT, wsT_d, writes=[n_wsT])
        DMA('sp', bsT, bsT_d, writes=[n_bsT])
        DMA('sp', cosT, cos_d, writes=[n_cos])
        DMA('sp', sinT, sin_d, writes=[n_sin])
        wblk = [A.tile([128, 16, 512], BF16, f"wblk{i}") for i in range(2)]
        hTl = [A.tile([128, 16, 128], BF16, f"hTl{i}") for i in range(6)]
        blkbuf = [A.tile([128, 16384], BF16, f"blkbuf{i}") for i in range(2)]
        ga_t = [A.tile([128, 512], F32, f"ga{i}") for i in range(2)]
        tmpf = [A.tile([128, 512], F32, f"tmpf{i}") for i in range(2)]
        tmpb = [A.tile([128, 512], BF16, f"tmpb{i}") for i in range(2)]
        oab = [A.tile([128, 256], BF16, f"oab{i}") for i in range(2)]
        sm = [A.tile([128, 64], F32, f"sm{i}") for i in range(2)]
        rot = [A.tile([128, 4, 8, 8], F32, f"rot{i}") for i in range(2)]
        w_in_v = w_in_d.rearrange("(kc p) n -> p kc n", p=128)

        def load_wblk(blk):
            w_t, n_w = wblk[blk % 2]
            for q4 in range(4):
                DMA('pool', w_t[:, q4 * 4:(q4 + 1) * 4, :], w_in_v[:, q4 * 4:(q4 + 1) * 4, blk * 512:(blk + 1) * 512],
                    writes=[n_w])

        work = []
        for blk in range(10):
            for t in range(NT_OWN if blk < 6 else NT_ALL):
                work.append((blk, t))

        def load_hT(i):
            blk, t = work[i]
            h_t, n_h = hTl[i % 6]
            DMA('sp', h_t, hT_s[t], reads=["hT_s"], writes=[n_h])

        def emit_mm(i):
            blk, t = work[i]
            if t == 0 and blk + 1 < 10:
                load_wblk(blk + 1)
            w_t, n_w = wblk[blk % 2]
            h_t, n_h = hTl[i % 6]
            zb = i % 4
            for kc in range(16):
                OP('pe', 'matmul', reads=[n_w, n_h], writes=[f"bank{zb}"], out=banks[zb][:, :], lhsT=h_t[:, kc, :],
                   rhs=w_t[:, kc, :], start=(kc == 0), stop=(kc == 15))

        def post(i):
            lst = []

            def Q(*a, **k):
                lst.append((OP, a, k))

            def QD(*a, **k):
                lst.append((DMA, a, k))
            blk, t = work[i]
            zb = i % 4
            pz, n_pz = banks[zb], f"bank{zb}"
            wsb, n_wsb = 4 + i % 2, f"bank{4 + i % 2}"
            trb, n_trb = 6 + i % 2, f"bank{6 + i % 2}"
            bb, n_bb = blkbuf[blk % 2]
            (ga, n_gat), (tf, n_tf), (tb, n_tb), (oa, n_oa), (s_, n_sm), (rt, n_rt) = \
                ga_t[i % 2], tmpf[i % 2], tmpb[i % 2], oab[i % 2], sm[i % 2], rot[i % 2]
            if blk < 4:
                h0 = 2 * blk
                Q('act', 'activation', reads=[n_pz], writes=[n_gat], out=ga, in_=pz[:, :], func=AF.Gelu_apprx_tanh)
                vv = ga[:, 256:512].rearrange("p (a b) -> p a b", a=2)
                tf3 = tf[:, 0:256].rearrange("p (a b) -> p a b", a=2)
                Q('dve', 'tensor_reduce', reads=[n_gat], writes=[n_sm], out=s_[:, 0:2], in_=vv, axis=AX.X, op=ALU.add)
                Q('dve', 'tensor_tensor', reads=[n_gat], writes=[n_tf], out=tf[:, 0:256], in0=ga[:, 256:512],
                   in1=ga[:, 256:512], op=ALU.mult)
                Q('dve', 'tensor_reduce', reads=[n_tf], writes=[n_sm], out=s_[:, 2:4], in_=tf3, axis=AX.X, op=ALU.add)
                Q('dve', 'tensor_scalar', reads=[n_sm], writes=[n_sm], out=s_[:, 4:6], in0=s_[:, 0:2], scalar1=1.0 / 128,
                   scalar2=None, op0=ALU.mult)
                Q('dve', 'tensor_tensor', reads=[n_sm], writes=[n_sm], out=s_[:, 6:8], in0=s_[:, 4:6], in1=s_[:, 4:6],
                   op=ALU.mult)
                Q('dve', 'scalar_tensor_tensor', reads=[n_sm], writes=[n_sm], out=s_[:, 8:10], in0=s_[:, 2:4],
                   scalar=1.0 / 128, in1=s_[:, 6:8], op0=ALU.mult, op1=ALU.subtract)
                Q('dve', 'tensor_scalar', reads=[n_sm], writes=[n_sm], out=s_[:, 8:10], in0=s_[:, 8:10], scalar1=EPS,
                   scalar2=None, op0=ALU.add)
                Q('pool', 'tensor_tensor', reads=[n_sm, n_nh], writes=[n_sm], out=s_[:, 10:12], in0=s_[:, 8:10],
                   in1=neghalf[:, 0:2], op=ALU.pow)
                Q('dve', 'tensor_tensor', reads=[n_gat, n_sm], writes=[n_tf], out=tf3, in0=vv,
                   in1=bc(s_[:, 4:6].unsqueeze(2), [128, 2, 128]), op=ALU.subtract)
                Q('dve', 'tensor_tensor', reads=[n_tf, n_sm], writes=[n_tf], out=tf3, in0=tf3,
                   in1=bc(s_[:, 10:12].unsqueeze(2), [128, 2, 128]), op=ALU.mult)
                Q('dve', 'tensor_tensor', reads=[n_tf, n_lng], writes=[n_tf], out=tf[:, 0:256], in0=tf[:, 0:256],
                   in1=lng[:, h0 * 128:(h0 + 2) * 128], op=ALU.mult)
                Q('dve', 'tensor_tensor', reads=[n_tf, n_lnb], writes=[n_tb], out=tb[:, 0:256], in0=tf[:, 0:256],
                   in1=lnb[:, h0 * 128:(h0 + 2) * 128], op=ALU.add)
                for hh in range(2):
                    Q('pe', 'matmul', reads=[n_wsT, n_tb], writes=[n_wsb], out=banks[wsb][:, hh * 128:(hh + 1) * 128],
                       lhsT=wsT[:, h0 + hh, :], rhs=tb[:, hh * 128:(hh + 1) * 128], start=True, stop=True)
                for hh in range(2):
                    Q('dve', 'scalar_tensor_tensor', reads=[n_wsb, n_bsT, n_gat], writes=[n_oa],
                       out=oa[:, hh * 128:(hh + 1) * 128], in0=banks[wsb][:, hh * 128:(hh + 1) * 128],
                       scalar=bsT[:, h0 + hh:h0 + hh + 1], in1=ga[:, hh * 128:(hh + 1) * 128], op0=ALU.add, op1=ALU.mult)
                for hh in range(2):
                    Q('pe', 'transpose', reads=[n_oa, n_ident], writes=[n_trb],
                       out=bank_bf16(trb)[:, hh * 128:(hh + 1) * 128], in_=oa[:, hh * 128:(hh + 1) * 128], identity=ident)
                mixblk = bb[:, 0:4096].rearrange("p (c t) -> p c t", c=2)
                Q('act', 'copy', reads=[n_trb], writes=[n_bb], out=mixblk[:, :, t * 128:(t + 1) * 128],
                   in_=bank_bf16(trb)[:, 0:256].rearrange("p (c t) -> p c t", c=2))
                if t == NT_OWN - 1:
                    QD('pool', mixT_s[h0:h0 + 2].rearrange("c p t -> p c t"), mixblk, reads=[n_bb], writes=["mixT_s"])
            elif blk < 8:
                isq = blk < 6
                j = (blk - 4) if isq else (blk - 6)
                gtile, n_g = (qg, n_qg) if isq else (kg, n_kg)
                ntile = NT_OWN if isq else NT_ALL
                Q('act', 'activation', reads=[n_pz], writes=[n_tf], out=tf, in_=pz[:, :], func=AF.Square)
                Q('dve', 'tensor_reduce', reads=[n_tf], writes=[n_sm], out=s_[:, 0:8],
                   in_=tf.rearrange("p (a b) -> p a b", a=8), axis=AX.X, op=ALU.add)
                Q('dve', 'tensor_scalar', reads=[n_sm], writes=[n_sm], out=s_[:, 8:16], in0=s_[:, 0:8], scalar1=1.0 / 64,
                   scalar2=EPS, op0=ALU.mult, op1=ALU.add)
                Q('pool', 'tensor_tensor', reads=[n_sm, n_nh], writes=[n_sm], out=s_[:, 16:24], in0=s_[:, 8:16],
                   in1=neghalf[:, 0:8], op=ALU.pow)
                ga3 = ga.rearrange("p (a b) -> p a b", a=8)
                Q('dve', 'tensor_tensor', reads=[n_pz, n_sm], writes=[n_gat], out=ga3,
                   in0=pz[:, :].rearrange("p (a b) -> p a b", a=8), in1=bc(s_[:, 16:24].unsqueeze(2), [128, 8, 64]),
                   op=ALU.mult)
                Q('dve', 'tensor_tensor', reads=[n_gat, n_g], writes=[n_gat], out=ga, in0=ga, in1=gtile, op=ALU.mult)
                x1v, x2v = ga3[:, :, 0:8], ga3[:, :, 8:16]
                cb = bc(cosT[:, t:t + 1, :], [128, 8, 8])
                sb_ = bc(sinT[:, t:t + 1, :], [128, 8, 8])
                Q('dve', 'tensor_tensor', reads=[n_gat, n_cos], writes=[n_rt], out=rt[:, 0], in0=x1v, in1=cb, op=ALU.mult)
                Q('dve', 'tensor_tensor', reads=[n_gat, n_sin], writes=[n_rt], out=rt[:, 1], in0=x2v, in1=sb_, op=ALU.mult)
                Q('dve', 'tensor_tensor', reads=[n_gat, n_cos], writes=[n_rt], out=rt[:, 2], in0=x2v, in1=cb, op=ALU.mult)
                Q('dve', 'tensor_tensor', reads=[n_gat, n_sin], writes=[n_rt], out=rt[:, 3], in0=x1v, in1=sb_, op=ALU.mult)
                Q('dve', 'tensor_tensor', reads=[n_rt], writes=[n_gat], out=x1v, in0=rt[:, 0], in1=rt[:, 1], op=ALU.subtract)
                Q('dve', 'tensor_tensor', reads=[n_rt], writes=[n_gat], out=x2v, in0=rt[:, 2], in1=rt[:, 3], op=ALU.add)
                Q('act', 'copy', reads=[n_gat], writes=[n_tb], out=tb, in_=ga)
                for hh in range(4):
                    Q('pe', 'transpose', reads=[n_tb, n_ident], writes=[n_trb],
                       out=bank_bf16(trb)[:, hh * 128:(hh + 1) * 128], in_=tb[:, hh * 128:(hh + 1) * 128], identity=ident)
                ntok = ntile * 128
                tblk = bb[:, 0:4 * ntok].rearrange("p (c t) -> p c t", c=4)
                Q('dve', 'tensor_copy', reads=[n_trb], writes=[n_bb], out=tblk[:, :, t * 128:(t + 1) * 128],
                   in_=bank_bf16(trb)[:, 0:512].rearrange("p (c t) -> p c t", c=4))
                if t == ntile - 1:
                    dst = qT_s if isq else kT_s
                    for hh in range(4):
                        QD('pool', dst[4 * j + hh], tblk[:, hh, :], reads=[n_bb], writes=["qk_s"])
            else:
                j = blk - 8
                vblk = bb.rearrange("p (t c e) -> p t c e", t=NT_ALL, c=4)
                Q('act', 'copy', reads=[n_pz], writes=[n_bb], out=vblk[:, t], in_=pz[:, :].rearrange("p (c e) -> p c e", c=4))
                if t == NT_ALL - 1:
                    for hh in range(4):
                        QD('pool', v_s[4 * j + hh], vblk[:, :, hh, :], reads=[n_bb], writes=["v_s"])
            return lst

        load_wblk(0)
        for i in range(4):
            load_hT(i)
        emit_mm(0)
        emit_mm(1)
        for p in range(len(work) // 2):
            for j in (2 * p + 2, 2 * p + 3):
                if j < len(work):
                    if j + 2 < len(work):
                        load_hT(j + 2)
                    emit_mm(j)
            l0, l1 = post(2 * p), post(2 * p + 1)
            for k in range(max(len(l0), len(l1))):
                for l in (l0, l1):
                    if k < len(l):
                        f_, a_, k_ = l[k]
                        f_(*a_, **k_)
        P.barrier()
        A.reset()

        subc, n_sub = A.tile([128, 1], F32, "subc")
        DMA('sp', subc, sub_d.rearrange("o e -> e o"), writes=[n_sub])
        onesf, n_onesf = A.tile([128, 128], F32, "onesf")
        OP('dve', 'memset', writes=[n_onesf], ap=onesf, constant=1.0)
        qTh = [A.tile([128, 2048], BF16, f"qTh{i}") for i in range(2)]
        kTh = [[A.tile([128, 4096], BF16, f"kTh{i}_{c}") for c in range(2)] for i in range(2)]
        vah = [A.tile([128, NT_ALL, 128], BF16, f"vah{i}") for i in range(2)]
        pT = [A.tile([128, 1024], BF16, f"pT{i}") for i in range(3)]
        accs = [A.tile([128, 1024], F32, f"acc{i}") for i in range(2)]
        rsum = [A.tile([128, 512], F32, f"rsum{i}") for i in range(2)]
        oc = [A.tile([128, 2048], F32, f"oc{i}") for i in range(2)]
        osq, n_osq = A.tile([128, 2048], F32, "osq")
        mixb = [A.tile([128, 2048], BF16, f"mixb{i}") for i in range(2)]
        for i in range(2):
            OP('dve', 'memset', writes=[kTh[i][0][1]], ap=kTh[i][0][0][64:128, :], constant=0.0)
            OP('dve', 'memset', writes=[kTh[i][1][1]], ap=kTh[i][1][0][0:64, :], constant=0.0)

        def load_head(h):
            DMA('sp', qTh[h % 2][0], qT_s[h], writes=[qTh[h % 2][1]])
            DMA('sp', kTh[h % 2][0][0][0:64, :], kT_s[h][0:64, :], writes=[kTh[h % 2][0][1]])
            DMA('sp', kTh[h % 2][1][0][64:128, :], kT_s[h][64:128, :], writes=[kTh[h % 2][1][1]])
            DMA('sp', vah[h % 2][0], v_s[h], writes=[vah[h % 2][1]])

        load_head(0)
        gidx = 0
        NP2 = NT_ALL // 2
        for h in range(8):
            if h + 1 < 8:
                load_head(h + 1)
            (q_t, n_q), (v_t, n_v) = qTh[h % 2], vah[h % 2]
            units = [(c, qb, kp) for c in range(2) for qb in range(4) for kp in range(NP2)]

            def emit_S(i):
                c, qb, kp = units[i]
                k_t, n_k = kTh[h % 2][c]
                b0 = (i % 2) * 2
                p_t, n_p = pT[i % 3]
                for j in range(2):
                    kt = kp * 2 + j
                    OP('pe', 'matmul', reads=[n_k, n_q], writes=[f"bank{b0 + j}"], out=banks[b0 + j][:, :],
                       lhsT=k_t[:, kt * 128:(kt + 1) * 128], rhs=q_t[:, qb * 512:(qb + 1) * 512], start=True, stop=True)
                OP('act', 'activation', reads=[f"bank{b0}", f"bank{b0 + 1}"], writes=[n_p],
                   out=p_t.rearrange("p (a b) -> p a b", a=2), in_=pair_view(b0), func=AF.Exp, scale=0.125)

            emit_S(0)
            for i, (c, qb, kp) in enumerate(units):
                if i + 1 < len(units):
                    emit_S(i + 1)
                g = gidx + i // NP2
                bo = 4 + g % 2
                p_t, n_p = pT[i % 3]
                a_t, n_a = accs[g % 2]
                for j in range(2):
                    kt = kp * 2 + j
                    OP('pe', 'matmul', reads=[n_v, n_p], writes=[f"bank{bo}"], out=banks[bo][:, :], lhsT=v_t[:, kt, :],
                       rhs=p_t[:, j * 512:(j + 1) * 512], start=(kt == 0), stop=(kt == NT_ALL - 1))
                if kp == 0:
                    OP('dve', 'tensor_copy', reads=[n_p], writes=[n_a], out=a_t, in_=p_t)
                else:
                    OP('dve', 'tensor_tensor', reads=[n_p, n_a], writes=[n_a], out=a_t, in0=a_t, in1=p_t, op=ALU.add)
                if kp == NP2 - 1:
                    bs_ = 6 + g % 2
                    r_t, n_r = rsum[g % 2]
                    o_t, n_o = oc[c]
                    for j in range(2):
                        OP('pe', 'matmul', reads=[n_onesf, n_a], writes=[f"bank{bs_}"], out=banks[bs_][:, :], lhsT=onesf,
                           rhs=a_t[:, j * 512:(j + 1) * 512], start=(j == 0), stop=(j == 1))
                    OP('dve', 'reciprocal', reads=[f"bank{bs_}"], writes=[n_r], out=r_t, in_=banks[bs_][:, :])
                    if c == 0:
                        OP('dve', 'tensor_tensor', reads=[f"bank{bo}", n_r], writes=[n_o], out=o_t[:, qb * 512:(qb + 1) * 512],
                           in0=banks[bo][:, :], in1=r_t, op=ALU.mult)
                    else:
                        OP('dve', 'scalar_tensor_tensor', reads=[f"bank{bo}", n_r, n_nl], writes=[n_o],
                           out=o_t[:, qb * 512:(qb + 1) * 512], in0=banks[bo][:, :], scalar=neg_lam[:, 0:1], in1=r_t,
                           op0=ALU.mult, op1=ALU.mult)
            gidx += 8
            (o0, n_o0), (o1, n_o1) = oc
            m_t, n_m = mixb[h % 2]
            OP('dve', 'tensor_tensor', reads=[n_o0, n_o1], writes=[n_o0], out=o0, in0=o0, in1=o1, op=ALU.add)
            OP('act', 'activation', reads=[n_o0], writes=[n_osq], out=osq, in_=o0, func=AF.Square)
            for nb in range(4):
                OP('pe', 'matmul', reads=[n_onesf, n_osq], writes=[f"bank{6 + nb % 2}"], out=banks[6 + nb % 2][:, :], lhsT=onesf,
                   rhs=osq[:, nb * 512:(nb + 1) * 512], start=True, stop=True)
                OP('dve', 'tensor_scalar', reads=[f"bank{6 + nb % 2}"], writes=[n_o1], out=o1[:, nb * 512:(nb + 1) * 512],
                   in0=banks[6 + nb % 2][:, :], scalar1=1.0 / 128, scalar2=EPS, op0=ALU.mult, op1=ALU.add)
            OP('act', 'activation', reads=[n_o1], writes=[n_o1], out=o1, in_=o1, func=AF.Sqrt)
            OP('dve', 'reciprocal', reads=[n_o1], writes=[n_o1], out=o1, in_=o1)
            OP('dve', 'tensor_tensor', reads=[n_o0, n_o1], writes=[n_osq], out=osq, in0=o0, in1=o1, op=ALU.mult)
            OP('dve', 'tensor_scalar', reads=[n_osq, n_sub], writes=[n_m], out=m_t, in0=osq, scalar1=subc[:, 0:1],
               scalar2=1.0 - LAMBDA_INIT, op0=ALU.mult, op1=ALU.mult)
            DMA('pool', mixT_s[8 + h], m_t, reads=[n_m], writes=["mixT_s"])
        P.barrier()
        A.reset()

        g_ffn, n_gf = A.tile([128, D], F32, "g_ffn")
        wr, n_wr = A.tile([128, 16, 36], F32, "wr")
        brt, n_br = A.tile([128, 36], F32, "br")
        wo, n_wo = A.tile([128, 16, D], BF16, "wo")
        zero_t, n_zero = A.tile([128, D], F32, "zero")
        DMA('sp', g_ffn, bc(g_ffn_d, [128, D]), writes=[n_gf])
        DMA('sp', wr, wr_d.rearrange("(kc p) n -> p kc n", p=128), writes=[n_wr])
        DMA('sp', brt, bc(br_d, [128, 36]), writes=[n_br])
        w_out_v = w_out_d.rearrange("(kc p) n -> p kc n", p=128)
        for q4 in range(8):
            DMA('pool', wo[:, q4 * 2:(q4 + 1) * 2, :], w_out_v[:, q4 * 2:(q4 + 1) * 2, :], writes=[n_wo])
        OP('dve', 'memset', writes=[n_zero], ap=zero_t, constant=0.0)
        DMA('sp', y_s[R_ROWS:R_ROWS + 128, :], zero_t, reads=[n_zero], writes=["y_trash"])
        mt = [A.tile([128, 16, 128], BF16, f"mt{i}") for i in range(2)]
        x3 = [A.tile([128, D], F32, f"x3{i}") for i in range(2)]
        x1t = [A.tile([128, D], F32, f"x1t{i}") for i in range(2)]
        hm, n_hm = A.tile([128, D], F32, "hm")
        hmb = [A.tile([128, D], BF16, f"hmb{i}") for i in range(2)]
        hmT, n_hmT = A.tile([128, 16, 128], F32, "hmT")
        rs_ = [A.tile([128, 256], F32, f"rs{i}") for i in range(2)]
        indb = [A.tile([128, 32], BF16, f"indb{i}") for i in range(2)]
        mixT_v = mixT_s.rearrange("c p t -> p c t")

        def load_s3(t):
            DMA('sp', mt[t % 2][0], mixT_v[:, :, t * 128:(t + 1) * 128], reads=["mixT_s"], writes=[mt[t % 2][1]])
            DMA('sp', x3[t % 2][0], x_d[t * 128:(t + 1) * 128, :], writes=[x3[t % 2][1]])

        load_s3(0)
        for t in range(NT_OWN):
            if t + 1 < NT_OWN:
                load_s3(t + 1)
            (m_t, n_m), (x_t, n_x), (x1, n_x1), (hb_t, n_hb), (r_, n_r), (ib, n_ib) = \
                mt[t % 2], x3[t % 2], x1t[t % 2], hmb[t % 2], rs_[t % 2], indb[t % 2]
            for nb in range(4):
                for c in range(16):
                    OP('pe', 'matmul', reads=[n_m, n_wo], writes=[f"bank{nb}"], out=banks[nb][:, :], lhsT=m_t[:, c, :],
                       rhs=wo[:, c, nb * 512:(nb + 1) * 512], start=(c == 0), stop=(c == 15))
            for nb in range(4):
                OP('dve', 'tensor_tensor', reads=[f"bank{nb}", n_x], writes=[n_x1], out=x1[:, nb * 512:(nb + 1) * 512],
                   in0=banks[nb][:, :], in1=x_t[:, nb * 512:(nb + 1) * 512], op=ALU.add)
            DMA('sp', x1_s[t], x1, reads=[n_x1], writes=["x1_s"])
            OP('act', 'activation', reads=[n_x1], writes=[n_hm, n_r], out=hm, in_=x1, func=AF.Square, accum_out=r_[:, 0:1])
            OP('act', 'activation', reads=[n_r], writes=[n_r], out=r_[:, 1:2], in_=r_[:, 0:1], func=AF.Sqrt, scale=1.0 / D,
               bias=EPS)
            OP('dve', 'reciprocal', reads=[n_r], writes=[n_r], out=r_[:, 1:2], in_=r_[:, 1:2])
            OP('dve', 'scalar_tensor_tensor', reads=[n_x1, n_r, n_gf], writes=[n_hm], out=hm, in0=x1, scalar=r_[:, 1:2],
               in1=g_ffn, op0=ALU.mult, op1=ALU.mult)
            OP('act', 'copy', reads=[n_hm], writes=[n_hb], out=hb_t, in_=hm)
            for c in range(16):
                bk = 4 + c // 4
                OP('pe', 'transpose', reads=[n_hm, n_identf], writes=[f"bank{bk}"],
                   out=banks[bk][:, (c % 4) * 128:(c % 4 + 1) * 128], in_=hm[:, c * 128:(c + 1) * 128], identity=identf)
            for q4 in range(4):
                OP('act' if q4 % 2 == 0 else 'dve', 'copy' if q4 % 2 == 0 else 'tensor_copy', reads=[f"bank{4 + q4}"],
                   writes=[n_hmT], out=hmT[:, q4 * 4:(q4 + 1) * 4, :],
                   in_=banks[4 + q4][:, :].rearrange("p (a b) -> p a b", a=4))
            for c in range(16):
                OP('pe', 'matmul', reads=[n_hmT, n_wr], writes=["bank0"], out=banks[0][:, 0:36], lhsT=hmT[:, c, :],
                   rhs=wr[:, c, :], start=(c == 0), stop=(c == 15))
            lg = r_[:, 8:44]
            gl = r_[:, 8:12]
            el = r_[:, 12:44].rearrange("p (g j) -> p g j", g=4)
            col = lambda a, n=1: r_[:, a:a + n]
            RW = dict(reads=[n_r], writes=[n_r])
            OP('dve', 'tensor_tensor', reads=["bank0", n_br], writes=[n_r], out=lg, in0=banks[0][:, 0:36], in1=brt, op=ALU.add)
            OP('dve', 'tensor_reduce', out=col(2), in_=gl, axis=AX.X, op=ALU.max, **RW)
            OP('dve', 'tensor_scalar', out=col(44, 4), in0=gl, scalar1=col(2), scalar2=None, op0=ALU.is_equal, **RW)
            OP('dve', 'tensor_scalar', out=col(3), in0=col(2), scalar1=-1.0, scalar2=None, op0=ALU.mult, **RW)
            OP('act', 'activation', out=col(48, 4), in_=gl, func=AF.Exp, bias=col(3), accum_out=col(4), **RW)
            OP('dve', 'reciprocal', out=col(5), in_=col(4), **RW)
            tmp48 = r_[:, 64:96].rearrange("p (g j) -> p g j", g=4)
            OP('dve', 'tensor_tensor', out=tmp48, in0=el, in1=bc(col(44, 4).unsqueeze(2), [128, 4, 8]), op=ALU.mult, **RW)
            OP('dve', 'tensor_reduce', out=col(96, 8), in_=r_[:, 64:96].rearrange("p (g j) -> p j g", g=4), axis=AX.X,
               op=ALU.add, **RW)
            OP('dve', 'tensor_reduce', out=col(6), in_=col(96, 8), axis=AX.X, op=ALU.max, **RW)
            OP('dve', 'tensor_scalar', out=col(104, 8), in0=col(96, 8), scalar1=col(6), scalar2=None, op0=ALU.is_equal, **RW)
            OP('dve', 'scalar_tensor_tensor', out=col(112, 8), in0=col(104, 8), scalar=-1e30, in1=col(96, 8), op0=ALU.mult,
               op1=ALU.add, **RW)
            OP('dve', 'tensor_reduce', out=col(7), in_=col(112, 8), axis=AX.X, op=ALU.max, **RW)
            OP('dve', 'tensor_scalar', out=col(120, 8), in0=col(112, 8), scalar1=col(7), scalar2=None, op0=ALU.is_equal, **RW)
            OP('dve', 'tensor_tensor', out=col(52), in0=col(7), in1=col(6), op=ALU.subtract, **RW)
            OP('act', 'activation', out=col(53), in_=col(52), func=AF.Exp, **RW)
            OP('dve', 'tensor_scalar', out=col(54), in0=col(53), scalar1=1.0, scalar2=None, op0=ALU.add, **RW)
            OP('dve', 'reciprocal', out=col(55), in_=col(54), **RW)
            OP('dve', 'tensor_tensor', out=col(56), in0=col(53), in1=col(55), op=ALU.mult, **RW)
            OP('dve', 'tensor_tensor', out=col(57), in0=col(55), in1=col(5), op=ALU.mult, **RW)
            OP('dve', 'tensor_tensor', out=col(58), in0=col(56), in1=col(5), op=ALU.mult, **RW)
            ind1 = r_[:, 128:160].rearrange("p (g j) -> p g j", g=4)
            ind2 = r_[:, 160:192].rearrange("p (g j) -> p g j", g=4)
            gohb = bc(col(44, 4).unsqueeze(2), [128, 4, 8])
            OP('dve', 'tensor_tensor', out=ind1, in0=bc(col(104, 8).unsqueeze(1), [128, 4, 8]), in1=gohb, op=ALU.mult, **RW)
            OP('dve', 'tensor_tensor', out=ind2, in0=bc(col(120, 8).unsqueeze(1), [128, 4, 8]), in1=gohb, op=ALU.mult, **RW)
            OP('dve', 'tensor_tensor', reads=[n_r], writes=[n_ib], out=ib, in0=col(128, 32), in1=col(160, 32), op=ALU.add)
            OP('pe', 'matmul', reads=[n_tri, n_ib], writes=["bank1"], out=banks[1][:, 0:32], lhsT=tri, rhs=ib, start=True,
               stop=True)
            OP('pe', 'matmul', reads=[n_ones, n_ib], writes=["bank1"], out=banks[1][:, 32:64], lhsT=ones, rhs=ib, start=True,
               stop=True)
            OP('dve', 'tensor_tensor', reads=["bank1", n_base, n_r], writes=[n_r], out=col(192, 32), in0=banks[1][:, 0:32],
               in1=base_bc, op=ALU.add)
            OP('dve', 'tensor_tensor', reads=["bank1", n_base], writes=[n_base], out=base_bc, in0=banks[1][:, 32:64],
               in1=base_bc, op=ALU.add)
            OP('dve', 'tensor_scalar', out=col(224, 32), in0=col(192, 32), scalar1=float(CAP), scalar2=None, op0=ALU.is_lt, **RW)
            OP('dve', 'tensor_tensor', reads=[n_r, n_eb], writes=[n_r], out=col(192, 32), in0=col(192, 32), in1=ebase, op=ALU.add)
            OP('dve', 'tensor_tensor', out=col(192, 32), in0=col(192, 32), in1=col(224, 32), op=ALU.mult, **RW)
            OP('dve', 'tensor_scalar', out=col(64, 32), in0=col(224, 32), scalar1=-1.0, scalar2=1.0, op0=ALU.mult, op1=ALU.add,
               **RW)
            OP('dve', 'scalar_tensor_tensor', reads=[n_r, n_trash], writes=[n_r], out=col(192, 32), in0=col(64, 32),
               scalar=trash[:, 0:1], in1=col(192, 32), op0=ALU.mult, op1=ALU.add)
            for k in range(2):
                indk = col(128 + 32 * k, 32)
                OP('dve', 'tensor_tensor', out=col(64, 32), in0=indk, in1=col(192, 32), op=ALU.mult, **RW)
                OP('dve', 'tensor_reduce', out=col(59 + k), in_=col(64, 32), axis=AX.X, op=ALU.add, **RW)
                OP('dve', 'tensor_tensor', out=col(64, 32), in0=indk, in1=col(224, 32), op=ALU.mult, **RW)
                OP('dve', 'tensor_reduce', out=col(61 + k), in_=col(64, 32), axis=AX.X, op=ALU.add, **RW)
                OP('dve', 'tensor_copy', reads=[n_r], writes=[n_idx + f"_{t}_{k}"], out=idx_all[:, t, k:k + 1], in_=col(59 + k))
                OP('dve', 'tensor_tensor', reads=[n_r], writes=[n_cw + f"_{t}"], out=cw_all[:, t, k:k + 1], in0=col(57 + k),
                   in1=col(61 + k), op=ALU.mult)
                P.dma('pool', (lambda tt, kk, src: (lambda e: e.indirect_dma_start(
                    out=xs_s, out_offset=bass.IndirectOffsetOnAxis(ap=idx_all[:, tt, kk:kk + 1], axis=0), in_=src,
                    in_offset=None)))(t, k, hb_t),
                    reads=[n_hb, n_idx + f"_{t}_{k}"], writes=["xs_s"])
        P.barrier()
        A.reset()

        NB = CAP // 128
        xtok = [A.tile([128, NB, D], BF16, f"xtok{i}") for i in range(2)]
        xbT, n_xbT = A.tile([128, 16, CAP], BF16, "xbT")
        wgu = [A.tile([128, 2, 16, 512], BF16, f"wgu{i}") for i in range(2)]
        wdp = [A.tile([128, 8, 512], BF16, f"wdp{i}") for i in range(3)]
        hT, n_hT = A.tile([128, 8, CAP], BF16, "hT")
        sg = [A.tile([128, CAP], F32, f"sg{i}") for i in range(2)]
        yt = [A.tile([128, D], F32, f"yt{i}") for i in range(NB)]
        wg_v = wg_d.rearrange("e (kc p) f -> e p kc f", p=128)
        wu_v = wu_d.rearrange("e (kc p) f -> e p kc f", p=128)
        wd_v = wd_d.rearrange("e (fc p) n -> e p fc n", p=128)
        xs_v = xs_s[0:R_ROWS, :].rearrange("(e b p) d -> e p b d", e=NE, p=128)
        y_v = y_s[0:R_ROWS, :].rearrange("(e b p) d -> e b p d", e=NE, p=128)

        def load_gu(e, fh):
            w_t, n_w = wgu[(e * 2 + fh) % 2]
            for m, src in enumerate((wg_v, wu_v)):
                for half in range(2):
                    DMA('pool', w_t[:, m, half * 8:(half + 1) * 8, :], src[e][:, half * 8:(half + 1) * 8, fh * 512:(fh + 1) * 512],
                        writes=[n_w])

        def load_wd(e, nb):
            w_t, n_w = wdp[(e * 4 + nb) % 3]
            DMA('pool', w_t, wd_v[e][:, :, nb * 512:(nb + 1) * 512], writes=[n_w])

        def load_xs(e):
            DMA('sp', xtok[e % 2][0], xs_v[e], reads=["xs_s"], writes=[xtok[e % 2][1]])

        seq = []
        for e in range(NE):
            seq += [('gu', e, 0), ('gu', e, 1), ('wd', e, 0), ('wd', e, 1), ('wd', e, 2), ('wd', e, 3)]
        issued = [0]

        def issue_until(n):
            while issued[0] < min(n, len(seq)):
                kind, e, a = seq[issued[0]]
                (load_gu if kind == 'gu' else load_wd)(e, a)
                issued[0] += 1

        load_xs(0)
        issue_until(2)
        gcnt = 0
        ycnt = 0
        step = 0
        for e in range(NE):
            if e + 1 < NE:
                load_xs(e + 1)
            x_t, n_x = xtok[e % 2]
            for kc in range(16):
                bk = 4 + kc % 2
                for b in range(NB):
                    OP('pe', 'transpose', reads=[n_x, n_ident], writes=[f"bank{bk}"],
                       out=bank_bf16(bk)[:, b * 128:(b + 1) * 128], in_=x_t[:, b, kc * 128:(kc + 1) * 128], identity=ident)
                OP('act' if kc % 2 == 0 else 'dve', 'copy' if kc % 2 == 0 else 'tensor_copy', reads=[f"bank{bk}"],
                   writes=[n_xbT + f"_{kc}"], out=xbT[:, kc, :], in_=bank_bf16(bk)[:, 0:CAP])
            xb_reads = [n_xbT + f"_{kc}" for kc in range(16)]
            for fh in range(2):
                issue_until(step + 3)
                step += 1
                w_t, n_w = wgu[(e * 2 + fh) % 2]
                for fcl in range(4):
                    fc = fh * 4 + fcl
                    bg = (gcnt % 2) * 2
                    s_t, n_s = sg[gcnt % 2]
                    gcnt += 1
                    for m in range(2):
                        for kc in range(16):
                            OP('pe', 'matmul', reads=[n_w, xb_reads[kc]], writes=[f"bank{bg + m}"], out=banks[bg + m][:, 0:CAP],
                               lhsT=w_t[:, m, kc, fcl * 128:(fcl + 1) * 128], rhs=xbT[:, kc, :], start=(kc == 0),
                               stop=(kc == 15))
                    OP('act', 'activation', reads=[f"bank{bg}"], writes=[n_s], out=s_t, in_=banks[bg][:, 0:CAP], func=AF.Silu)
                    OP('dve', 'tensor_tensor', reads=[n_s, f"bank{bg + 1}"], writes=[n_hT + f"_{fc}"], out=hT[:, fc, :],
                       in0=s_t, in1=banks[bg + 1][:, 0:CAP], op=ALU.mult)
            h_reads = [n_hT + f"_{fc}" for fc in range(8)]
            for nb in range(4):
                issue_until(step + 3)
                step += 1
                w_t, n_w = wdp[(e * 4 + nb) % 3]
                for b in range(NB):
                    by = 6 + ycnt % 2
                    ycnt += 1
                    for fc in range(8):
                        OP('pe', 'matmul', reads=[n_w, h_reads[fc]], writes=[f"bank{by}"], out=banks[by][:, :],
                           lhsT=hT[:, fc, b * 128:(b + 1) * 128], rhs=w_t[:, fc, :], start=(fc == 0), stop=(fc == 7))
                    OP('act' if ycnt % 2 == 0 else 'dve', 'copy' if ycnt % 2 == 0 else 'tensor_copy', reads=[f"bank{by}"],
                       writes=[yt[b][1]], out=yt[b][0][:, nb * 512:(nb + 1) * 512], in_=banks[by][:, :])
            for b in range(NB):
                DMA('sp', y_v[e][b], yt[b][0], reads=[yt[b][1]], writes=["y_s"])
        P.barrier()
        A.reset()

        ya = [A.tile([128, D], F32, f"ya{i}") for i in range(2)]
        yb = [A.tile([128, D], F32, f"yb{i}") for i in range(2)]
        xo = [A.tile([128, D], F32, f"xo{i}") for i in range(2)]
        for t in range(NT_OWN):
            (a_t, n_a), (b_t, n_b), (o_t, n_o) = ya[t % 2], yb[t % 2], xo[t % 2]
            for k, (dst, n_dst) in enumerate(((a_t, n_a), (b_t, n_b))):
                P.dma('pool', (lambda tt, kk, d_: (lambda e: e.indirect_dma_start(
                    out=d_, out_offset=None, in_=y_s, in_offset=bass.IndirectOffsetOnAxis(ap=idx_all[:, tt, kk:kk + 1], axis=0))))(t, k, dst), reads=["y_s"], writes=[n_dst])
            DMA('sp', o_t, x1_s[t], reads=["x1_s"], writes=[n_o])
            OP('dve', 'scalar_tensor_tensor', reads=[n_a, n_o], writes=[n_o], out=o_t, in0=a_t, scalar=cw_all[:, t, 0:1],
               in1=o_t, op0=ALU.mult, op1=ALU.add)
            OP('dve', 'scalar_tensor_tensor', reads=[n_b, n_o], writes=[n_o], out=o_t, in0=b_t, scalar=cw_all[:, t, 1:2],
               in1=o_t, op0=ALU.mult, op1=ALU.add)
            DMA('sp', out_d[t * 128:(t + 1) * 128, :], o_t, reads=[n_o], writes=["out"])
        P.finish()
        P.emit()
    return nc


def _rot_tables():
    pos = np.arange(SEQ, dtype=np.float32)
    inv = (1.0 / (np.float32(500000.0) ** (np.arange(0, 16, 2, dtype=np.float32) / np.float32(16)))).astype(np.float32)
    ang = (pos[:, None] * inv[None, :]).astype(np.float32)
    return np.cos(ang).astype(np.float32), np.sin(ang).astype(np.float32)


def make_in_maps(inp):
    f = lambda a: np.ascontiguousarray(np.asarray(a, dtype=np.float32))
    x = f(inp["x"])
    w_in = f(inp["w_in"])[0]
    perm = []
    for j in range(4):
        for h in (2 * j, 2 * j + 1):
            perm += list(range(h * 128, (h + 1) * 128))
        for h in (2 * j, 2 * j + 1):
            perm += list(range(1024 + h * 128, 1024 + (h + 1) * 128))
    perm += list(range(2048, 5120))
    w_in_p = np.ascontiguousarray(w_in[:, perm])
    cos, sin = _rot_tables()
    shared = dict(
        g_attn=f(inp["attn_norm_g"]).reshape(1, D), w_in=w_in_p,
        ln_g=f(inp["gmlp_ln_g"]).reshape(1, 1024), ln_b=f(inp["gmlp_ln_b"]).reshape(1, 1024),
        wsT=np.ascontiguousarray(f(inp["gmlp_ws"])[0].transpose(2, 0, 1)),
        bsT=np.ascontiguousarray(f(inp["gmlp_bs"])[0].T),
        qg=np.ascontiguousarray(np.tile(f(inp["q_norm_g"]).reshape(1, 64), (1, 8))),
        kg=np.ascontiguousarray(np.tile(f(inp["k_norm_g"]).reshape(1, 64), (1, 8))),
        lamv=np.concatenate([f(inp[k]).reshape(1, 64) for k in ("lambda_q1", "lambda_q2", "lambda_k1", "lambda_k2")], axis=1),
        subg=f(inp["subln_g"]).reshape(1, 128), w_out=f(inp["w_out"])[0], g_ffn=f(inp["ffn_norm_g"]).reshape(1, D),
        wr=np.ascontiguousarray(np.concatenate([f(inp["w_group"])[0], f(inp["w_router"])[0]], axis=1)),
        br=np.concatenate([f(inp["b_group"]).reshape(1, 4), f(inp["b_router"]).reshape(1, 32)], axis=1),
        w_gate=f(inp["w_gate"])[0], w_up=f(inp["w_up"])[0], w_down=f(inp["w_down"])[0],
    )
    maps = []
    for c in range(8):
        b, s = c // 2, c % 2
        own = slice(s * 2048, (s + 1) * 2048)
        oth = slice((1 - s) * 2048, (2 - s) * 2048)
        xc = np.concatenate([x[b, own], x[b, oth]], axis=0)
        cc = np.concatenate([cos[own], cos[oth]], axis=0).reshape(NT_ALL, 128, 8).transpose(1, 0, 2)
        sc = np.concatenate([sin[own], sin[oth]], axis=0).reshape(NT_ALL, 128, 8).transpose(1, 0, 2)
        m = dict(shared)
        m.update(x=np.ascontiguousarray(xc), cos=np.ascontiguousarray(cc), sin=np.ascontiguousarray(sc))
        maps.append(m)
    return maps


def kernel(**inputs):
    nc = build_program()
    in_maps = make_in_maps(inputs)
    res = run_bass_kernel_spmd(nc, in_maps, core_ids=list(range(8)))
    out = np.empty((4, SEQ, D), np.float32)
    for c in range(8):
        b, s = c // 2, c % 2
        out[b, s * 2048:(s + 1) * 2048] = res.results[c]["out"]
    return out
```

```python
import contextlib
import math
import numpy as np
import concourse.bass as bass
import concourse.mybir as mybir
from concourse.bass_utils import run_bass_kernel_spmd

F32 = mybir.dt.float32
BF16 = mybir.dt.bfloat16
I32 = mybir.dt.int32
U8 = mybir.dt.uint8
ALU = mybir.AluOpType
AF = mybir.ActivationFunctionType
AX = mybir.AxisListType

D = 2048
SEQ = 4096
NT_OWN = 16
NT_ALL = 32
CAP = 384
NE = 32
R_ROWS = NE * CAP
EPS = 1e-6
LAMBDA_INIT = 0.8 - 0.6 * math.exp(0.0)


class Prog:
    ENG = ('pe', 'dve', 'act', 'pool', 'sp')
    EPOCH = 8000
    NDMA = 8

    def __init__(self, nc, es):
        self.nc = nc
        self.es = es
        self.ops = {e: [] for e in self.ENG}
        self.lastw = {}
        self.readers = {}
        self.dma_n = {q: 0 for q in self.ENG}
        self.dma_last = {}

    def _deps(self, reads, writes):
        deps = {}

        def add(key, val):
            if deps.get(key, -1) < val:
                deps[key] = val
        for r in reads:
            if r in self.lastw:
                add(*self.lastw[r])
        for w in writes:
            if w in self.lastw:
                add(*self.lastw[w])
            for k, v in self.readers.get(w, {}).items():
                add(k, v)
        return deps

    def _update(self, tok, reads, writes):
        k, v = tok
        for r in reads:
            d = self.readers.setdefault(r, {})
            if d.get(k, -1) < v:
                d[k] = v
        for w in writes:
            self.lastw[w] = tok
            self.readers[w] = {}

    def op(self, eng, fn, reads=(), writes=()):
        idx = len(self.ops[eng])
        tok = (('op', eng), idx)
        deps = self._deps(reads, writes)
        if eng == 'pe':
            deps.pop(('op', 'pe'), None)
        self.ops[eng].append(dict(fn=fn, deps=deps, kind='op', flagged=False))
        self._update(tok, reads, writes)
        return tok

    def dma(self, q, fn, reads=(), writes=()):
        n = self.dma_n[q]
        self.dma_n[q] += 1
        slot = n % self.NDMA
        val = 16 * (n // self.NDMA + 1)
        tok = (('dma', q, slot), val)
        deps = self._deps(reads, writes)
        prev = self.dma_last.get((q, slot))
        if prev is not None and deps.get(prev[0], -1) < prev[1]:
            deps[prev[0]] = prev[1]
        self.dma_last[(q, slot)] = tok
        self.ops[q].append(dict(fn=fn, deps=deps, kind='dma', slot=slot))
        self._update(tok, reads, writes)
        return tok

    def _all_deps(self):
        deps = {}
        for (q, slot), tok in self.dma_last.items():
            deps[tok[0]] = tok[1]
        for e in self.ENG:
            if e == 'sp':
                continue
            for i in range(len(self.ops[e]) - 1, -1, -1):
                ent = self.ops[e][i]
                if ent['fn'] is not None and ent['kind'] == 'op':
                    deps[('op', e)] = i
                    break
        return deps

    def barrier(self):
        idx = len(self.ops['sp'])
        self.ops['sp'].append(dict(fn=lambda e: e.nop(), deps=self._all_deps(), kind='op', flagged=True))
        for e in self.ENG:
            if e != 'sp':
                self.ops[e].append(dict(fn=None, deps={('op', 'sp'): idx}, kind='op', flagged=False))
        self.lastw = {}
        self.readers = {}

    def finish(self):
        self.ops['sp'].append(dict(fn=None, deps=self._all_deps(), kind='op', flagged=False))

    def emit(self):
        nc, es = self.nc, self.es
        for e in self.ENG:
            for ent in self.ops[e]:
                for key, val in ent['deps'].items():
                    if key[0] == 'op':
                        self.ops[key[1]][val]['flagged'] = True
        self.cnt = {}
        self.esems = {}
        for e in self.ENG:
            c = 0
            arr = []
            for ent in self.ops[e]:
                if ent['kind'] == 'op' and ent['flagged'] and ent['fn'] is not None:
                    c += 1
                arr.append(c)
            self.cnt[e] = arr
            nep = (c + self.EPOCH - 1) // self.EPOCH
            self.esems[e] = [es.enter_context(nc.semaphore(f"s_{e}_{i}")) for i in range(max(nep, 1))]
        self.dsems = {}
        for q in self.ENG:
            if self.dma_n[q]:
                self.dsems[q] = [es.enter_context(nc.semaphore(f"d_{q}_{i}")) for i in range(self.NDMA)]
        block = es.enter_context(nc.Block())
        block.tensor(lambda e: self._replay('pe', e))
        block.vector(lambda e: self._replay('dve', e))
        block.scalar(lambda e: self._replay('act', e))
        block.gpsimd(lambda e: self._replay('pool', e))
        block.sync(lambda e: self._replay('sp', e))

    def _replay(self, E, eng):
        waited = {}
        for i, ent in enumerate(self.ops[E]):
            for key, val in ent['deps'].items():
                if key[0] == 'op':
                    c = self.cnt[key[1]][val]
                    if c == 0 or waited.get(key, 0) >= c:
                        continue
                    waited[key] = c
                    eng.wait_ge(self.esems[key[1]][(c - 1) // self.EPOCH], (c - 1) % self.EPOCH + 1)
                else:
                    if waited.get(key, 0) >= val:
                        continue
                    waited[key] = val
                    eng.wait_ge(self.dsems[key[1]][key[2]], val)
            if ent['fn'] is None:
                continue
            ins = ent['fn'](eng)
            if ent['kind'] == 'dma':
                ins.then_inc(self.dsems[E][ent['slot']], 16)
            elif ent['flagged']:
                c = self.cnt[E][i]
                ins.then_inc(self.esems[E][(c - 1) // self.EPOCH], 1)


class Arena:
    def __init__(self, nc, es, nbytes):
        self.t = es.enter_context(nc.sbuf_tensor("arena", [128, nbytes], U8))
        self.nbytes = nbytes
        self.off = 0
        self.base = 0
        self.n = 0

    def tile(self, shape, dt, name=None):
        sz = {F32: 4, BF16: 2, I32: 4}[dt]
        n = int(np.prod(shape[1:])) * sz
        n = (n + 63) // 64 * 64
        assert self.off + n <= self.nbytes, (self.off, n, self.nbytes, name)
        v = self.t[:, self.off:self.off + n]
        if n != int(np.prod(shape[1:])) * sz:
            v = self.t[:, self.off:self.off + int(np.prod(shape[1:])) * sz]
        v = v.bitcast(dt)
        if len(shape) == 3:
            v = v.rearrange("p (a b) -> p a b", a=shape[1])
        elif len(shape) == 4:
            v = v.rearrange("p (a b c) -> p a b c", a=shape[1], b=shape[2])
        self.off += n
        self.n += 1
        return v, (name or "t") + f"#{self.n}"

    def mark(self):
        self.base = self.off

    def reset(self):
        self.off = self.base


def build_program(debug_outs=()):
    nc = bass.Bass("TRN2", target_bir_lowering=False)
    es = contextlib.ExitStack()

    def din(name, shape, dt=F32):
        return nc.dram_tensor(name, shape, dt, kind="ExternalInput").ap()

    def dscr(name, shape, dt):
        kind = "ExternalOutput" if name in debug_outs else "Internal"
        return nc.dram_tensor(name, shape, dt, kind=kind).ap()

    x_d = din("x", [SEQ, D])
    cos_d = din("cos", [128, NT_ALL, 8])
    sin_d = din("sin", [128, NT_ALL, 8])
    g_attn_d = din("g_attn", [1, D])
    w_in_d = din("w_in", [D, 5120])
    lng_d = din("ln_g", [1, 1024])
    lnb_d = din("ln_b", [1, 1024])
    wsT_d = din("wsT", [128, 8, 128])
    bsT_d = din("bsT", [128, 8])
    qg_d = din("qg", [1, 512])
    kg_d = din("kg", [1, 512])
    lam_d = din("lamv", [1, 256])
    sub_d = din("subg", [1, 128])
    w_out_d = din("w_out", [D, D])
    g_ffn_d = din("g_ffn", [1, D])
    wr_d = din("wr", [D, 36])
    br_d = din("br", [1, 36])
    wg_d = din("w_gate", [NE, D, 1024])
    wu_d = din("w_up", [NE, D, 1024])
    wd_d = din("w_down", [NE, 1024, D])
    out_d = nc.dram_tensor("out", [NT_OWN * 128, D], F32, kind="ExternalOutput").ap()

    hT_s = dscr("hT_s", [NT_ALL, 128, 16, 128], BF16)
    qT_s = dscr("qT_s", [8, 128, 2048], BF16)
    kT_s = dscr("kT_s", [8, 128, 4096], BF16)
    v_s = dscr("v_s", [8, 128, NT_ALL, 128], BF16)
    mixT_s = dscr("mixT_s", [16, 128, 2048], BF16)
    x1_s = dscr("x1_s", [NT_OWN, 128, D], F32)
    xs_s = dscr("xs_s", [R_ROWS + 128, D], BF16)
    y_s = dscr("y_s", [R_ROWS + 128, D], F32)

    with es:
        P = Prog(nc, es)
        A = Arena(nc, es, 204 * 1024)
        psum_all = es.enter_context(nc.psum_tensor("psum_all", [128, 4096], F32))
        banks = [psum_all[:, i * 512:(i + 1) * 512] for i in range(8)]

        def OP(eng, meth, reads=(), writes=(), **kw):
            P.op(eng, lambda e: getattr(e, meth)(**kw), reads, writes)

        def DMA(q, out, in_, reads=(), writes=()):
            P.dma(q, lambda e: e.dma_start(out=out, in_=in_), reads, writes)

        def bc(ap, shape):
            return ap.to_broadcast(shape)

        ident, n_ident = A.tile([128, 128], BF16, "ident")
        identf, n_identf = A.tile([128, 128], F32, "identf")
        tri, n_tri = A.tile([128, 128], BF16, "tri")
        ones, n_ones = A.tile([128, 128], BF16, "ones")
        neghalf, n_nh = A.tile([128, 32], F32, "neghalf")
        ebase, n_eb = A.tile([128, 32], F32, "ebase")
        trash, n_trash = A.tile([128, 1], F32, "trash")
        neg_lam, n_nl = A.tile([128, 1], F32, "neg_lam")
        base_bc, n_base = A.tile([128, 32], F32, "base_bc")
        idx_all, n_idx = A.tile([128, NT_OWN, 2], I32, "idx_all")
        cw_all, n_cw = A.tile([128, NT_OWN, 2], F32, "cw_all")
        lamt, n_lamt = A.tile([128, 256], F32, "lamt")
        lamp, n_lamp = A.tile([128, 128], F32, "lamp")
        lams, n_lams = A.tile([128, 2], F32, "lams")
        A.mark()

        OP('pool', 'iota', writes=[n_identf], out=identf, pattern=[[1, 128]], base=0, channel_multiplier=-1,
           allow_small_or_imprecise_dtypes=True)
        OP('dve', 'tensor_scalar', reads=[n_identf], writes=[n_ident], out=ident, in0=identf, scalar1=0.0, scalar2=None,
           op0=ALU.is_equal)
        OP('dve', 'tensor_scalar', reads=[n_identf], writes=[n_tri], out=tri, in0=identf, scalar1=0.0, scalar2=None,
           op0=ALU.is_gt)
        OP('dve', 'tensor_scalar', reads=[n_identf, n_ident, n_tri], writes=[n_identf], out=identf, in0=identf, scalar1=0.0,
           scalar2=None, op0=ALU.is_equal)
        OP('dve', 'memset', writes=[n_ones], ap=ones, constant=1.0)
        OP('dve', 'memset', writes=[n_nh], ap=neghalf, constant=-0.5)
        OP('dve', 'memset', writes=[n_base], ap=base_bc, constant=0.0)
        OP('pool', 'iota', writes=[n_eb], out=ebase, pattern=[[CAP, 32]], base=0, channel_multiplier=0,
           allow_small_or_imprecise_dtypes=True)
        OP('pool', 'iota', writes=[n_trash], out=trash, pattern=[[1, 1]], base=R_ROWS, channel_multiplier=1,
           allow_small_or_imprecise_dtypes=True)
        DMA('sp', lamt, bc(lam_d, [128, 256]), writes=[n_lamt])
        OP('dve', 'tensor_tensor', reads=[n_lamt], writes=[n_lamp], out=lamp, in0=lamt[:, 0:128], in1=lamt[:, 128:256],
           op=ALU.mult)
        OP('dve', 'tensor_reduce', reads=[n_lamp], writes=[n_lams], out=lams, in_=lamp.rearrange("p (a b) -> p a b", a=2),
           axis=AX.X, op=ALU.add)
        OP('act', 'activation', reads=[n_lams], writes=[n_lams], out=lams, in_=lams, func=AF.Exp)
        OP('dve', 'tensor_tensor', reads=[n_lams], writes=[n_nl], out=neg_lam, in0=lams[:, 1:2], in1=lams[:, 0:1],
           op=ALU.subtract)
        OP('dve', 'tensor_scalar', reads=[n_nl], writes=[n_nl], out=neg_lam, in0=neg_lam, scalar1=-LAMBDA_INIT, scalar2=None,
           op0=ALU.add)

        def pair_view(i):
            return psum_all[:, i * 512:(i + 2) * 512].rearrange("p (a b) -> p a b", a=2)

        def bank_bf16(i):
            return banks[i][:, :].bitcast(BF16)

        g_attn, n_ga = A.tile([128, D], F32, "g_attn")
        DMA('sp', g_attn, bc(g_attn_d, [128, D]), writes=[n_ga])
        xt = [A.tile([128, D], F32, f"xt{i}") for i in range(2)]
        junk, n_junk = A.tile([128, D], BF16, "junk")
        hb = [A.tile([128, D], BF16, f"hb{i}") for i in range(2)]
        hTt = [A.tile([128, D], BF16, f"hTt{i}") for i in range(2)]
        st = [A.tile([128, 2], F32, f"st{i}") for i in range(2)]
        for t in range(NT_ALL):
            b = t % 2
            (x_t, n_x), (h_t, n_h), (hT_t, n_hT), (s_t, n_s) = xt[b], hb[b], hTt[b], st[b]
            DMA('sp', x_t, x_d[t * 128:(t + 1) * 128, :], writes=[n_x])
            OP('act', 'activation', reads=[n_x], writes=[n_junk, n_s], out=junk, in_=x_t, func=AF.Square,
               accum_out=s_t[:, 0:1])
            OP('act', 'activation', reads=[n_s], writes=[n_s], out=s_t[:, 1:2], in_=s_t[:, 0:1], func=AF.Sqrt,
               scale=1.0 / D, bias=EPS)
            OP('dve', 'reciprocal', reads=[n_s], writes=[n_s], out=s_t[:, 1:2], in_=s_t[:, 1:2])
            OP('dve', 'scalar_tensor_tensor', reads=[n_x, n_s, n_ga], writes=[n_h], out=h_t, in0=x_t, scalar=s_t[:, 1:2],
               in1=g_attn, op0=ALU.mult, op1=ALU.mult)
            for kc in range(16):
                bk = 6 + kc // 8
                OP('pe', 'transpose', reads=[n_h, n_ident], writes=[f"bank{bk}"],
                   out=bank_bf16(bk)[:, (kc % 8) * 128:(kc % 8 + 1) * 128], in_=h_t[:, kc * 128:(kc + 1) * 128],
                   identity=ident)
            OP('act', 'copy', reads=["bank6"], writes=[n_hT], out=hT_t[:, 0:1024], in_=bank_bf16(6))
            OP('dve', 'tensor_copy', reads=["bank7"], writes=[n_hT], out=hT_t[:, 1024:2048], in_=bank_bf16(7))
            DMA('pool', hT_s[t], hT_t.rearrange("p (a b) -> p a b", a=16), reads=[n_hT], writes=["hT_s"])
        P.barrier()
        A.reset()

        lng, n_lng = A.tile([128, 1024], F32, "lng")
        lnb, n_lnb = A.tile([128, 1024], F32, "lnb")
        qg, n_qg = A.tile([128, 512], F32, "qg")
        kg, n_kg = A.tile([128, 512], F32, "kg")
        wsT, n_wsT = A.tile([128, 8, 128], BF16, "wsT")
        bsT, n_bsT = A.tile([128, 8], F32, "bsT")
        cosT, n_cos = A.tile([128, NT_ALL, 8], F32, "cosT")
        sinT, n_sin = A.tile([128, NT_ALL, 8], F32, "sinT")
        DMA('sp', lng, bc(lng_d, [128, 1024]), writes=[n_lng])
        DMA('sp', lnb, bc(lnb_d, [128, 1024]), writes=[n_lnb])
        DMA('sp', qg, bc(qg_d, [128, 512]), writes=[n_qg])
        DMA('sp', kg, bc(kg_d, [128, 512]), writes=[n_kg])
        DMA('pool', wsT, wsT_d, writes=[n_wsT])
        DMA('sp', bsT, bsT_d, writes=[n_bsT])
        DMA('sp', cosT, cos_d, writes=[n_cos])
        DMA('sp', sinT, sin_d, writes=[n_sin])
        wblk = [A.tile([128, 16, 512], BF16, f"wblk{i}") for i in range(2)]
        hTl = [A.tile([128, 16, 128], BF16, f"hTl{i}") for i in range(6)]
        blkbuf = [A.tile([128, 16384], BF16, f"blkbuf{i}") for i in range(2)]
        ga_t = [A.tile([128, 512], F32, f"ga{i}") for i in range(2)]
        tmpf = [A.tile([128, 512], F32, f"tmpf{i}") for i in range(2)]
        tmpb = [A.tile([128, 512], BF16, f"tmpb{i}") for i in range(2)]
        oab = [A.tile([128, 256], BF16, f"oab{i}") for i in range(2)]
        sm = [A.tile([128, 64], F32, f"sm{i}") for i in range(2)]
        rot = [A.tile([128, 4, 8, 8], F32, f"rot{i}") for i in range(2)]
        w_in_v = w_in_d.rearrange("(kc p) n -> p kc n", p=128)

        def load_wblk(blk):
            w_t, n_w = wblk[blk % 2]
            for q4 in range(4):
                DMA('pool', w_t[:, q4 * 4:(q4 + 1) * 4, :], w_in_v[:, q4 * 4:(q4 + 1) * 4, blk * 512:(blk + 1) * 512],
                    writes=[n_w])

        work = []
        for blk in range(10):
            for t in range(NT_OWN if blk < 6 else NT_ALL):
                work.append((blk, t))

        def load_hT(i):
            blk, t = work[i]
            h_t, n_h = hTl[i % 6]
            DMA('sp', h_t, hT_s[t], reads=["hT_s"], writes=[n_h])

        def emit_mm(i):
            blk, t = work[i]
            if t == 0 and blk + 1 < 10:
                load_wblk(blk + 1)
            w_t, n_w = wblk[blk % 2]
            h_t, n_h = hTl[i % 6]
            zb = i % 4
            for kc in range(16):
                OP('pe', 'matmul', reads=[n_w, n_h], writes=[f"bank{zb}"], out=banks[zb][:, :], lhsT=h_t[:, kc, :],
                   rhs=w_t[:, kc, :], start=(kc == 0), stop=(kc == 15))

        def post(i):
            lst = []

            def Q(*a, **k):
                lst.append((OP, a, k))

            def QD(*a, **k):
                lst.append((DMA, a, k))
            blk, t = work[i]
            zb = i % 4
            pz, n_pz = banks[zb], f"bank{zb}"
            wsb, n_wsb = 4 + i % 2, f"bank{4 + i % 2}"
            trb, n_trb = 6 + i % 2, f"bank{6 + i % 2}"
            bb, n_bb = blkbuf[blk % 2]
            (ga, n_gat), (tf, n_tf), (tb, n_tb), (oa, n_oa), (s_, n_sm), (rt, n_rt) = \
                ga_t[i % 2], tmpf[i % 2], tmpb[i % 2], oab[i % 2], sm[i % 2], rot[i % 2]
            if blk < 4:
                h0 = 2 * blk
                Q('act', 'activation', reads=[n_pz], writes=[n_gat], out=ga, in_=pz[:, :], func=AF.Gelu_apprx_tanh)
                vv = ga[:, 256:512].rearrange("p (a b) -> p a b", a=2)
                tf3 = tf[:, 0:256].rearrange("p (a b) -> p a b", a=2)
                Q('dve', 'tensor_reduce', reads=[n_gat], writes=[n_sm], out=s_[:, 0:2], in_=vv, axis=AX.X, op=ALU.add)
                Q('dve', 'tensor_tensor', reads=[n_gat], writes=[n_tf], out=tf[:, 0:256], in0=ga[:, 256:512],
                   in1=ga[:, 256:512], op=ALU.mult)
                Q('dve', 'tensor_reduce', reads=[n_tf], writes=[n_sm], out=s_[:, 2:4], in_=tf3, axis=AX.X, op=ALU.add)
                Q('dve', 'tensor_scalar', reads=[n_sm], writes=[n_sm], out=s_[:, 4:6], in0=s_[:, 0:2], scalar1=1.0 / 128,
                   scalar2=None, op0=ALU.mult)
                Q('dve', 'tensor_tensor', reads=[n_sm], writes=[n_sm], out=s_[:, 6:8], in0=s_[:, 4:6], in1=s_[:, 4:6],
                   op=ALU.mult)
                Q('dve', 'scalar_tensor_tensor', reads=[n_sm], writes=[n_sm], out=s_[:, 8:10], in0=s_[:, 2:4],
                   scalar=1.0 / 128, in1=s_[:, 6:8], op0=ALU.mult, op1=ALU.subtract)
                Q('dve', 'tensor_scalar', reads=[n_sm], writes=[n_sm], out=s_[:, 8:10], in0=s_[:, 8:10], scalar1=EPS,
                   scalar2=None, op0=ALU.add)
                Q('pool', 'tensor_tensor', reads=[n_sm, n_nh], writes=[n_sm], out=s_[:, 10:12], in0=s_[:, 8:10],
                   in1=neghalf[:, 0:2], op=ALU.pow)
                Q('dve', 'tensor_tensor', reads=[n_gat, n_sm], writes=[n_tf], out=tf3, in0=vv,
                   in1=bc(s_[:, 4:6].unsqueeze(2), [128, 2, 128]), op=ALU.subtract)
                Q('dve', 'tensor_tensor', reads=[n_tf, n_sm], writes=[n_tf], out=tf3, in0=tf3,
                   in1=bc(s_[:, 10:12].unsqueeze(2), [128, 2, 128]), op=ALU.mult)
                Q('dve', 'tensor_tensor', reads=[n_tf, n_lng], writes=[n_tf], out=tf[:, 0:256], in0=tf[:, 0:256],
                   in1=lng[:, h0 * 128:(h0 + 2) * 128], op=ALU.mult)
                Q('dve', 'tensor_tensor', reads=[n_tf, n_lnb], writes=[n_tb], out=tb[:, 0:256], in0=tf[:, 0:256],
                   in1=lnb[:, h0 * 128:(h0 + 2) * 128], op=ALU.add)
                for hh in range(2):
                    Q('pe', 'matmul', reads=[n_wsT, n_tb], writes=[n_wsb], out=banks[wsb][:, hh * 128:(hh + 1) * 128],
                       lhsT=wsT[:, h0 + hh, :], rhs=tb[:, hh * 128:(hh + 1) * 128], start=True, stop=True)
                for hh in range(2):
                    Q('dve', 'scalar_tensor_tensor', reads=[n_wsb, n_bsT, n_gat], writes=[n_oa],
                       out=oa[:, hh * 128:(hh + 1) * 128], in0=banks[wsb][:, hh * 128:(hh + 1) * 128],
                       scalar=bsT[:, h0 + hh:h0 + hh + 1], in1=ga[:, hh * 128:(hh + 1) * 128], op0=ALU.add, op1=ALU.mult)
                for hh in range(2):
                    Q('pe', 'transpose', reads=[n_oa, n_ident], writes=[n_trb],
                       out=bank_bf16(trb)[:, hh * 128:(hh + 1) * 128], in_=oa[:, hh * 128:(hh + 1) * 128], identity=ident)
                mixblk = bb[:, 0:4096].rearrange("p (c t) -> p c t", c=2)
                Q('act', 'copy', reads=[n_trb], writes=[n_bb], out=mixblk[:, :, t * 128:(t + 1) * 128],
                   in_=bank_bf16(trb)[:, 0:256].rearrange("p (c t) -> p c t", c=2))
                if t == NT_OWN - 1:
                    QD('pool', mixT_s[h0:h0 + 2].rearrange("c p t -> p c t"), mixblk, reads=[n_bb], writes=["mixT_s"])
            elif blk < 8:
                isq = blk < 6
                j = (blk - 4) if isq else (blk - 6)
                gtile, n_g = (qg, n_qg) if isq else (kg, n_kg)
                ntile = NT_OWN if isq else NT_ALL
                Q('act', 'activation', reads=[n_pz], writes=[n_tf], out=tf, in_=pz[:, :], func=AF.Square)
                Q('dve', 'tensor_reduce', reads=[n_tf], writes=[n_sm], out=s_[:, 0:8],
                   in_=tf.rearrange("p (a b) -> p a b", a=8), axis=AX.X, op=ALU.add)
                Q('dve', 'tensor_scalar', reads=[n_sm], writes=[n_sm], out=s_[:, 8:16], in0=s_[:, 0:8], scalar1=1.0 / 64,
                   scalar2=EPS, op0=ALU.mult, op1=ALU.add)
                Q('pool', 'tensor_tensor', reads=[n_sm, n_nh], writes=[n_sm], out=s_[:, 16:24], in0=s_[:, 8:16],
                   in1=neghalf[:, 0:8], op=ALU.pow)
                ga3 = ga.rearrange("p (a b) -> p a b", a=8)
                Q('dve', 'tensor_tensor', reads=[n_pz, n_sm], writes=[n_gat], out=ga3,
                   in0=pz[:, :].rearrange("p (a b) -> p a b", a=8), in1=bc(s_[:, 16:24].unsqueeze(2), [128, 8, 64]),
                   op=ALU.mult)
                Q('dve', 'tensor_tensor', reads=[n_gat, n_g], writes=[n_gat], out=ga, in0=ga, in1=gtile, op=ALU.mult)
                x1v, x2v = ga3[:, :, 0:8], ga3[:, :, 8:16]
                cb = bc(cosT[:, t:t + 1, :], [128, 8, 8])
                sb_ = bc(sinT[:, t:t + 1, :], [128, 8, 8])
                Q('dve', 'tensor_tensor', reads=[n_gat, n_cos], writes=[n_rt], out=rt[:, 0], in0=x1v, in1=cb, op=ALU.mult)
                Q('dve', 'tensor_tensor', reads=[n_gat, n_sin], writes=[n_rt], out=rt[:, 1], in0=x2v, in1=sb_, op=ALU.mult)
                Q('dve', 'tensor_tensor', reads=[n_gat, n_cos], writes=[n_rt], out=rt[:, 2], in0=x2v, in1=cb, op=ALU.mult)
                Q('dve', 'tensor_tensor', reads=[n_gat, n_sin], writes=[n_rt], out=rt[:, 3], in0=x1v, in1=sb_, op=ALU.mult)
                Q('dve', 'tensor_tensor', reads=[n_rt], writes=[n_gat], out=x1v, in0=rt[:, 0], in1=rt[:, 1], op=ALU.subtract)
                Q('dve', 'tensor_tensor', reads=[n_rt], writes=[n_gat], out=x2v, in0=rt[:, 2], in1=rt[:, 3], op=ALU.add)
                Q('act', 'copy', reads=[n_gat], writes=[n_tb], out=tb, in_=ga)
                for hh in range(4):
                    Q('pe', 'transpose', reads=[n_tb, n_ident], writes=[n_trb],
                       out=bank_bf16(trb)[:, hh * 128:(hh + 1) * 128], in_=tb[:, hh * 128:(hh + 1) * 128], identity=ident)
                ntok = ntile * 128
                tblk = bb[:, 0:4 * ntok].rearrange("p (c t) -> p c t", c=4)
                Q('dve', 'tensor_copy', reads=[n_trb], writes=[n_bb], out=tblk[:, :, t * 128:(t + 1) * 128],
                   in_=bank_bf16(trb)[:, 0:512].rearrange("p (c t) -> p c t", c=4))
                if t == ntile - 1:
                    dst = qT_s if isq else kT_s
                    for hh in range(4):
                        QD('pool', dst[4 * j + hh], tblk[:, hh, :], reads=[n_bb], writes=["qk_s"])
            else:
                j = blk - 8
                vblk = bb.rearrange("p (t c e) -> p t c e", t=NT_ALL, c=4)
                Q('act', 'copy', reads=[n_pz], writes=[n_bb], out=vblk[:, t], in_=pz[:, :].rearrange("p (c e) -> p c e", c=4))
                if t == NT_ALL - 1:
                    for hh in range(4):
                        QD('pool', v_s[4 * j + hh], vblk[:, :, hh, :], reads=[n_bb], writes=["v_s"])
            return lst

        load_wblk(0)
        for i in range(4):
            load_hT(i)
        emit_mm(0)
        emit_mm(1)
        for p in range(len(work) // 2):
            for j in (2 * p + 2, 2 * p + 3):
                if j < len(work):
                    if j + 2 < len(work):
                        load_hT(j + 2)
                    emit_mm(j)
            l0, l1 = post(2 * p), post(2 * p + 1)
            for k in range(max(len(l0), len(l1))):
                for l in (l0, l1):
                    if k < len(l):
                        f_, a_, k_ = l[k]
                        f_(*a_, **k_)
        P.barrier()
        A.reset()

        subc, n_sub = A.tile([128, 1], F32, "subc")
        DMA('sp', subc, sub_d.rearrange("o e -> e o"), writes=[n_sub])
        onesf, n_onesf = A.tile([128, 128], F32, "onesf")
        OP('dve', 'memset', writes=[n_onesf], ap=onesf, constant=1.0)
        qTh = [A.tile([128, 2048], BF16, f"qTh{i}") for i in range(2)]
        kTh = [[A.tile([128, 4096], BF16, f"kTh{i}_{c}") for c in range(2)] for i in range(2)]
        vah = [A.tile([128, NT_ALL, 128], BF16, f"vah{i}") for i in range(2)]
        pT = [A.tile([128, 1024], BF16, f"pT{i}") for i in range(3)]
        accs = [A.tile([128, 1024], F32, f"acc{i}") for i in range(2)]
        rsum = [A.tile([128, 512], F32, f"rsum{i}") for i in range(2)]
        oc = [A.tile([128, 2048], F32, f"oc{i}") for i in range(2)]
        osq, n_osq = A.tile([128, 2048], F32, "osq")
        mixb = [A.tile([128, 2048], BF16, f"mixb{i}") for i in range(2)]
        for i in range(2):
            OP('dve', 'memset', writes=[kTh[i][0][1]], ap=kTh[i][0][0][64:128, :], constant=0.0)
            OP('dve', 'memset', writes=[kTh[i][1][1]], ap=kTh[i][1][0][0:64, :], constant=0.0)

        def load_head(h):
            DMA('sp', qTh[h % 2][0], qT_s[h], writes=[qTh[h % 2][1]])
            DMA('sp', kTh[h % 2][0][0][0:64, :], kT_s[h][0:64, :], writes=[kTh[h % 2][0][1]])
            DMA('sp', kTh[h % 2][1][0][64:128, :], kT_s[h][64:128, :], writes=[kTh[h % 2][1][1]])
            DMA('sp', vah[h % 2][0], v_s[h], writes=[vah[h % 2][1]])

        load_head(0)
        gidx = 0
        NP2 = NT_ALL // 2
        for h in range(8):
            if h + 1 < 8:
                load_head(h + 1)
            (q_t, n_q), (v_t, n_v) = qTh[h % 2], vah[h % 2]
            units = [(c, qb, kp) for c in range(2) for qb in range(4) for kp in range(NP2)]

            def emit_S(i):
                c, qb, kp = units[i]
                k_t, n_k = kTh[h % 2][c]
                b0 = (i % 2) * 2
                p_t, n_p = pT[i % 3]
                for j in range(2):
                    kt = kp * 2 + j
                    OP('pe', 'matmul', reads=[n_k, n_q], writes=[f"bank{b0 + j}"], out=banks[b0 + j][:, :],
                       lhsT=k_t[:, kt * 128:(kt + 1) * 128], rhs=q_t[:, qb * 512:(qb + 1) * 512], start=True, stop=True)
                OP('act', 'activation', reads=[f"bank{b0}", f"bank{b0 + 1}"], writes=[n_p],
                   out=p_t.rearrange("p (a b) -> p a b", a=2), in_=pair_view(b0), func=AF.Exp, scale=0.125)

            emit_S(0)
            for i, (c, qb, kp) in enumerate(units):
                if i + 1 < len(units):
                    emit_S(i + 1)
                g = gidx + i // NP2
                bo = 4 + g % 2
                p_t, n_p = pT[i % 3]
                a_t, n_a = accs[g % 2]
                for j in range(2):
                    kt = kp * 2 + j
                    OP('pe', 'matmul', reads=[n_v, n_p], writes=[f"bank{bo}"], out=banks[bo][:, :], lhsT=v_t[:, kt, :],
                       rhs=p_t[:, j * 512:(j + 1) * 512], start=(kt == 0), stop=(kt == NT_ALL - 1))
                if kp == 0:
                    OP('dve', 'tensor_copy', reads=[n_p], writes=[n_a], out=a_t, in_=p_t)
                else:
                    OP('dve', 'tensor_tensor', reads=[n_p, n_a], writes=[n_a], out=a_t, in0=a_t, in1=p_t, op=ALU.add)
                if kp == NP2 - 1:
                    bs_ = 6 + g % 2
                    r_t, n_r = rsum[g % 2]
                    o_t, n_o = oc[c]
                    for j in range(2):
                        OP('pe', 'matmul', reads=[n_onesf, n_a], writes=[f"bank{bs_}"], out=banks[bs_][:, :], lhsT=onesf,
                           rhs=a_t[:, j * 512:(j + 1) * 512], start=(j == 0), stop=(j == 1))
                    OP('dve', 'reciprocal', reads=[f"bank{bs_}"], writes=[n_r], out=r_t, in_=banks[bs_][:, :])
                    if c == 0:
                        OP('dve', 'tensor_tensor', reads=[f"bank{bo}", n_r], writes=[n_o], out=o_t[:, qb * 512:(qb + 1) * 512],
                           in0=banks[bo][:, :], in1=r_t, op=ALU.mult)
                    else:
                        OP('dve', 'scalar_tensor_tensor', reads=[f"bank{bo}", n_r, n_nl], writes=[n_o],
                           out=o_t[:, qb * 512:(qb + 1) * 512], in0=banks[bo][:, :], scalar=neg_lam[:, 0:1], in1=r_t,
                           op0=ALU.mult, op1=ALU.mult)
            gidx += 8
            (o0, n_o0), (o1, n_o1) = oc
            m_t, n_m = mixb[h % 2]
            OP('dve', 'tensor_tensor', reads=[n_o0, n_o1], writes=[n_o0], out=o0, in0=o0, in1=o1, op=ALU.add)
            OP('act', 'activation', reads=[n_o0], writes=[n_osq], out=osq, in_=o0, func=AF.Square)
            for nb in range(4):
                OP('pe', 'matmul', reads=[n_onesf, n_osq], writes=[f"bank{6 + nb % 2}"], out=banks[6 + nb % 2][:, :], lhsT=onesf,
                   rhs=osq[:, nb * 512:(nb + 1) * 512], start=True, stop=True)
                OP('dve', 'tensor_scalar', reads=[f"bank{6 + nb % 2}"], writes=[n_o1], out=o1[:, nb * 512:(nb + 1) * 512],
                   in0=banks[6 + nb % 2][:, :], scalar1=1.0 / 128, scalar2=EPS, op0=ALU.mult, op1=ALU.add)
            OP('act', 'activation', reads=[n_o1], writes=[n_o1], out=o1, in_=o1, func=AF.Sqrt)
            OP('dve', 'reciprocal', reads=[n_o1], writes=[n_o1], out=o1, in_=o1)
            OP('dve', 'tensor_tensor', reads=[n_o0, n_o1], writes=[n_osq], out=osq, in0=o0, in1=o1, op=ALU.mult)
            OP('dve', 'tensor_scalar', reads=[n_osq, n_sub], writes=[n_m], out=m_t, in0=osq, scalar1=subc[:, 0:1],
               scalar2=1.0 - LAMBDA_INIT, op0=ALU.mult, op1=ALU.mult)
            DMA('pool', mixT_s[8 + h], m_t, reads=[n_m], writes=["mixT_s"])
        P.barrier()
        A.reset()

        g_ffn, n_gf = A.tile([128, D], F32, "g_ffn")
        wr, n_wr = A.tile([128, 16, 36], F32, "wr")
        brt, n_br = A.tile([128, 36], F32, "br")
        wo, n_wo = A.tile([128, 16, D], BF16, "wo")
        zero_t, n_zero = A.tile([128, D], F32, "zero")
        DMA('sp', g_ffn, bc(g_ffn_d, [128, D]), writes=[n_gf])
        DMA('sp', wr, wr_d.rearrange("(kc p) n -> p kc n", p=128), writes=[n_wr])
        DMA('sp', brt, bc(br_d, [128, 36]), writes=[n_br])
        w_out_v = w_out_d.rearrange("(kc p) n -> p kc n", p=128)
        for q4 in range(8):
            DMA('pool', wo[:, q4 * 2:(q4 + 1) * 2, :], w_out_v[:, q4 * 2:(q4 + 1) * 2, :], writes=[n_wo])
        OP('dve', 'memset', writes=[n_zero], ap=zero_t, constant=0.0)
        DMA('sp', y_s[R_ROWS:R_ROWS + 128, :], zero_t, reads=[n_zero], writes=["y_trash"])
        mt = [A.tile([128, 16, 128], BF16, f"mt{i}") for i in range(2)]
        x3 = [A.tile([128, D], F32, f"x3{i}") for i in range(2)]
        x1t = [A.tile([128, D], F32, f"x1t{i}") for i in range(2)]
        hm, n_hm = A.tile([128, D], F32, "hm")
        hmb = [A.tile([128, D], BF16, f"hmb{i}") for i in range(2)]
        hmT, n_hmT = A.tile([128, 16, 128], F32, "hmT")
        rs_ = [A.tile([128, 256], F32, f"rs{i}") for i in range(2)]
        indb = [A.tile([128, 32], BF16, f"indb{i}") for i in range(2)]
        mixT_v = mixT_s.rearrange("c p t -> p c t")

        def load_s3(t):
            DMA('sp', mt[t % 2][0], mixT_v[:, :, t * 128:(t + 1) * 128], reads=["mixT_s"], writes=[mt[t % 2][1]])
            DMA('sp', x3[t % 2][0], x_d[t * 128:(t + 1) * 128, :], writes=[x3[t % 2][1]])

        load_s3(0)
        for t in range(NT_OWN):
            if t + 1 < NT_OWN:
                load_s3(t + 1)
            (m_t, n_m), (x_t, n_x), (x1, n_x1), (hb_t, n_hb), (r_, n_r), (ib, n_ib) = \
                mt[t % 2], x3[t % 2], x1t[t % 2], hmb[t % 2], rs_[t % 2], indb[t % 2]
            for nb in range(4):
                for c in range(16):
                    OP('pe', 'matmul', reads=[n_m, n_wo], writes=[f"bank{nb}"], out=banks[nb][:, :], lhsT=m_t[:, c, :],
                       rhs=wo[:, c, nb * 512:(nb + 1) * 512], start=(c == 0), stop=(c == 15))
            for nb in range(4):
                OP('dve', 'tensor_tensor', reads=[f"bank{nb}", n_x], writes=[n_x1], out=x1[:, nb * 512:(nb + 1) * 512],
                   in0=banks[nb][:, :], in1=x_t[:, nb * 512:(nb + 1) * 512], op=ALU.add)
            DMA('sp', x1_s[t], x1, reads=[n_x1], writes=["x1_s"])
            OP('act', 'activation', reads=[n_x1], writes=[n_hm, n_r], out=hm, in_=x1, func=AF.Square, accum_out=r_[:, 0:1])
            OP('act', 'activation', reads=[n_r], writes=[n_r], out=r_[:, 1:2], in_=r_[:, 0:1], func=AF.Sqrt, scale=1.0 / D,
               bias=EPS)
            OP('dve', 'reciprocal', reads=[n_r], writes=[n_r], out=r_[:, 1:2], in_=r_[:, 1:2])
            OP('dve', 'scalar_tensor_tensor', reads=[n_x1, n_r, n_gf], writes=[n_hm], out=hm, in0=x1, scalar=r_[:, 1:2],
               in1=g_ffn, op0=ALU.mult, op1=ALU.mult)
            OP('act', 'copy', reads=[n_hm], writes=[n_hb], out=hb_t, in_=hm)
            for c in range(16):
                bk = 4 + c // 4
                OP('pe', 'transpose', reads=[n_hm, n_identf], writes=[f"bank{bk}"],
                   out=banks[bk][:, (c % 4) * 128:(c % 4 + 1) * 128], in_=hm[:, c * 128:(c + 1) * 128], identity=identf)
            for q4 in range(4):
                OP('act' if q4 % 2 == 0 else 'dve', 'copy' if q4 % 2 == 0 else 'tensor_copy', reads=[f"bank{4 + q4}"],
                   writes=[n_hmT], out=hmT[:, q4 * 4:(q4 + 1) * 4, :],
                   in_=banks[4 + q4][:, :].rearrange("p (a b) -> p a b", a=4))
            for c in range(16):
                OP('pe', 'matmul', reads=[n_hmT, n_wr], writes=["bank0"], out=banks[0][:, 0:36], lhsT=hmT[:, c, :],
                   rhs=wr[:, c, :], start=(c == 0), stop=(c == 15))
            lg = r_[:, 8:44]
            gl = r_[:, 8:12]
            el = r_[:, 12:44].rearrange("p (g j) -> p g j", g=4)
            col = lambda a, n=1: r_[:, a:a + n]
            RW = dict(reads=[n_r], writes=[n_r])
            OP('dve', 'tensor_tensor', reads=["bank0", n_br], writes=[n_r], out=lg, in0=banks[0][:, 0:36], in1=brt, op=ALU.add)
            OP('dve', 'tensor_reduce', out=col(2), in_=gl, axis=AX.X, op=ALU.max, **RW)
            OP('dve', 'tensor_scalar', out=col(44, 4), in0=gl, scalar1=col(2), scalar2=None, op0=ALU.is_equal, **RW)
            OP('dve', 'tensor_scalar', out=col(3), in0=col(2), scalar1=-1.0, scalar2=None, op0=ALU.mult, **RW)
            OP('act', 'activation', out=col(48, 4), in_=gl, func=AF.Exp, bias=col(3), accum_out=col(4), **RW)
            OP('dve', 'reciprocal', out=col(5), in_=col(4), **RW)
            tmp48 = r_[:, 64:96].rearrange("p (g j) -> p g j", g=4)
            OP('dve', 'tensor_tensor', out=tmp48, in0=el, in1=bc(col(44, 4).unsqueeze(2), [128, 4, 8]), op=ALU.mult, **RW)
            OP('dve', 'tensor_reduce', out=col(96, 8), in_=r_[:, 64:96].rearrange("p (g j) -> p j g", g=4), axis=AX.X,
               op=ALU.add, **RW)
            OP('dve', 'tensor_reduce', out=col(6), in_=col(96, 8), axis=AX.X, op=ALU.max, **RW)
            OP('dve', 'tensor_scalar', out=col(104, 8), in0=col(96, 8), scalar1=col(6), scalar2=None, op0=ALU.is_equal, **RW)
            OP('dve', 'scalar_tensor_tensor', out=col(112, 8), in0=col(104, 8), scalar=-1e30, in1=col(96, 8), op0=ALU.mult,
               op1=ALU.add, **RW)
            OP('dve', 'tensor_reduce', out=col(7), in_=col(112, 8), axis=AX.X, op=ALU.max, **RW)
            OP('dve', 'tensor_scalar', out=col(120, 8), in0=col(112, 8), scalar1=col(7), scalar2=None, op0=ALU.is_equal, **RW)
            OP('dve', 'tensor_tensor', out=col(52), in0=col(7), in1=col(6), op=ALU.subtract, **RW)
            OP('act', 'activation', out=col(53), in_=col(52), func=AF.Exp, **RW)
            OP('dve', 'tensor_scalar', out=col(54), in0=col(53), scalar1=1.0, scalar2=None, op0=ALU.add, **RW)
            OP('dve', 'reciprocal', out=col(55), in_=col(54), **RW)
            OP('dve', 'tensor_tensor', out=col(56), in0=col(53), in1=col(55), op=ALU.mult, **RW)
            OP('dve', 'tensor_tensor', out=col(57), in0=col(55), in1=col(5), op=ALU.mult, **RW)
            OP('dve', 'tensor_tensor', out=col(58), in0=col(56), in1=col(5), op=ALU.mult, **RW)
            ind1 = r_[:, 128:160].rearrange("p (g j) -> p g j", g=4)
            ind2 = r_[:, 160:192].rearrange("p (g j) -> p g j", g=4)
            gohb = bc(col(44, 4).unsqueeze(2), [128, 4, 8])
            OP('dve', 'tensor_tensor', out=ind1, in0=bc(col(104, 8).unsqueeze(1), [128, 4, 8]), in1=gohb, op=ALU.mult, **RW)
            OP('dve', 'tensor_tensor', out=ind2, in0=bc(col(120, 8).unsqueeze(1), [128, 4, 8]), in1=gohb, op=ALU.mult, **RW)
            OP('dve', 'tensor_tensor', reads=[n_r], writes=[n_ib], out=ib, in0=col(128, 32), in1=col(160, 32), op=ALU.add)
            OP('pe', 'matmul', reads=[n_tri, n_ib], writes=["bank1"], out=banks[1][:, 0:32], lhsT=tri, rhs=ib, start=True,
               stop=True)
            OP('pe', 'matmul', reads=[n_ones, n_ib], writes=["bank1"], out=banks[1][:, 32:64], lhsT=ones, rhs=ib, start=True,
               stop=True)
            OP('dve', 'tensor_tensor', reads=["bank1", n_base, n_r], writes=[n_r], out=col(192, 32), in0=banks[1][:, 0:32],
               in1=base_bc, op=ALU.add)
            OP('dve', 'tensor_tensor', reads=["bank1", n_base], writes=[n_base], out=base_bc, in0=banks[1][:, 32:64],
               in1=base_bc, op=ALU.add)
            OP('dve', 'tensor_scalar', out=col(224, 32), in0=col(192, 32), scalar1=float(CAP), scalar2=None, op0=ALU.is_lt, **RW)
            OP('dve', 'tensor_tensor', reads=[n_r, n_eb], writes=[n_r], out=col(192, 32), in0=col(192, 32), in1=ebase, op=ALU.add)
            OP('dve', 'tensor_tensor', out=col(192, 32), in0=col(192, 32), in1=col(224, 32), op=ALU.mult, **RW)
            OP('dve', 'tensor_scalar', out=col(64, 32), in0=col(224, 32), scalar1=-1.0, scalar2=1.0, op0=ALU.mult, op1=ALU.add,
               **RW)
            OP('dve', 'scalar_tensor_tensor', reads=[n_r, n_trash], writes=[n_r], out=col(192, 32), in0=col(64, 32),
               scalar=trash[:, 0:1], in1=col(192, 32), op0=ALU.mult, op1=ALU.add)
            for k in range(2):
                indk = col(128 + 32 * k, 32)
                OP('dve', 'tensor_tensor', out=col(64, 32), in0=indk, in1=col(192, 32), op=ALU.mult, **RW)
                OP('dve', 'tensor_reduce', out=col(59 + k), in_=col(64, 32), axis=AX.X, op=ALU.add, **RW)
                OP('dve', 'tensor_tensor', out=col(64, 32), in0=indk, in1=col(224, 32), op=ALU.mult, **RW)
                OP('dve', 'tensor_reduce', out=col(61 + k), in_=col(64, 32), axis=AX.X, op=ALU.add, **RW)
                OP('dve', 'tensor_copy', reads=[n_r], writes=[n_idx + f"_{t}_{k}"], out=idx_all[:, t, k:k + 1], in_=col(59 + k))
                OP('dve', 'tensor_tensor', reads=[n_r], writes=[n_cw + f"_{t}"], out=cw_all[:, t, k:k + 1], in0=col(57 + k),
                   in1=col(61 + k), op=ALU.mult)
                P.dma('pool', (lambda tt, kk, src: (lambda e: e.indirect_dma_start(
                    out=xs_s, out_offset=bass.IndirectOffsetOnAxis(ap=idx_all[:, tt, kk:kk + 1], axis=0), in_=src,
                    in_offset=None)))(t, k, hb_t),
                    reads=[n_hb, n_idx + f"_{t}_{k}"], writes=["xs_s"])
        P.barrier()
        A.reset()

        NB = CAP // 128
        xtok = [A.tile([128, NB, D], BF16, f"xtok{i}") for i in range(2)]
        xbT, n_xbT = A.tile([128, 16, CAP], BF16, "xbT")
        wgu = [A.tile([128, 2, 16, 512], BF16, f"wgu{i}") for i in range(3)]
        wdp = [A.tile([128, 8, 512], BF16, f"wdp{i}") for i in range(4)]
        hT, n_hT = A.tile([128, 8, CAP], BF16, "hT")
        sg = [A.tile([128, CAP], F32, f"sg{i}") for i in range(2)]
        yt = [A.tile([128, D], F32, f"yt{i}") for i in range(NB)]
        wg_v = wg_d.rearrange("e (kc p) f -> e p kc f", p=128)
        wu_v = wu_d.rearrange("e (kc p) f -> e p kc f", p=128)
        wd_v = wd_d.rearrange("e (fc p) n -> e p fc n", p=128)
        xs_v = xs_s[0:R_ROWS, :].rearrange("(e b p) d -> e p b d", e=NE, p=128)
        y_v = y_s[0:R_ROWS, :].rearrange("(e b p) d -> e b p d", e=NE, p=128)

        def load_gu(e, fh):
            w_t, n_w = wgu[(e * 2 + fh) % 3]
            for m, src in enumerate((wg_v, wu_v)):
                for half in range(2):
                    DMA('pool', w_t[:, m, half * 8:(half + 1) * 8, :], src[e][:, half * 8:(half + 1) * 8, fh * 512:(fh + 1) * 512],
                        writes=[n_w])

        def load_wd(e, nb):
            w_t, n_w = wdp[(e * 4 + nb) % 4]
            DMA('pool', w_t, wd_v[e][:, :, nb * 512:(nb + 1) * 512], writes=[n_w])

        def load_xs(e):
            DMA('sp', xtok[e % 2][0], xs_v[e], reads=["xs_s"], writes=[xtok[e % 2][1]])

        seq = []
        for e in range(NE):
            seq += [('gu', e, 0), ('gu', e, 1), ('wd', e, 0), ('wd', e, 1), ('wd', e, 2), ('wd', e, 3)]
        issued = [0]

        def issue_until(n):
            while issued[0] < min(n, len(seq)):
                kind, e, a = seq[issued[0]]
                (load_gu if kind == 'gu' else load_wd)(e, a)
                issued[0] += 1

        load_xs(0)
        issue_until(3)
        gcnt = 0
        ycnt = 0
        step = 0
        for e in range(NE):
            if e + 1 < NE:
                load_xs(e + 1)
            x_t, n_x = xtok[e % 2]
            for kc in range(16):
                bk = 4 + kc % 2
                for b in range(NB):
                    OP('pe', 'transpose', reads=[n_x, n_ident], writes=[f"bank{bk}"],
                       out=bank_bf16(bk)[:, b * 128:(b + 1) * 128], in_=x_t[:, b, kc * 128:(kc + 1) * 128], identity=ident)
                OP('act' if kc % 2 == 0 else 'dve', 'copy' if kc % 2 == 0 else 'tensor_copy', reads=[f"bank{bk}"],
                   writes=[n_xbT + f"_{kc}"], out=xbT[:, kc, :], in_=bank_bf16(bk)[:, 0:CAP])
            xb_reads = [n_xbT + f"_{kc}" for kc in range(16)]
            for fh in range(2):
                issue_until(step + 4)
                step += 1
                w_t, n_w = wgu[(e * 2 + fh) % 3]
                for fcl in range(4):
                    fc = fh * 4 + fcl
                    bg = (gcnt % 2) * 2
                    s_t, n_s = sg[gcnt % 2]
                    gcnt += 1
                    for m in range(2):
                        for kc in range(16):
                            OP('pe', 'matmul', reads=[n_w, xb_reads[kc]], writes=[f"bank{bg + m}"], out=banks[bg + m][:, 0:CAP],
                               lhsT=w_t[:, m, kc, fcl * 128:(fcl + 1) * 128], rhs=xbT[:, kc, :], start=(kc == 0),
                               stop=(kc == 15))
                    OP('act', 'activation', reads=[f"bank{bg}"], writes=[n_s], out=s_t, in_=banks[bg][:, 0:CAP], func=AF.Silu)
                    OP('dve', 'tensor_tensor', reads=[n_s, f"bank{bg + 1}"], writes=[n_hT + f"_{fc}"], out=hT[:, fc, :],
                       in0=s_t, in1=banks[bg + 1][:, 0:CAP], op=ALU.mult)
            h_reads = [n_hT + f"_{fc}" for fc in range(8)]
            for nb in range(4):
                issue_until(step + 4)
                step += 1
                w_t, n_w = wdp[(e * 4 + nb) % 4]
                for b in range(NB):
                    by = 6 + ycnt % 2
                    ycnt += 1
                    for fc in range(8):
                        OP('pe', 'matmul', reads=[n_w, h_reads[fc]], writes=[f"bank{by}"], out=banks[by][:, :],
                           lhsT=hT[:, fc, b * 128:(b + 1) * 128], rhs=w_t[:, fc, :], start=(fc == 0), stop=(fc == 7))
                    OP('act' if ycnt % 2 == 0 else 'dve', 'copy' if ycnt % 2 == 0 else 'tensor_copy', reads=[f"bank{by}"],
                       writes=[yt[b][1]], out=yt[b][0][:, nb * 512:(nb + 1) * 512], in_=banks[by][:, :])
            for b in range(NB):
                DMA('sp', y_v[e][b], yt[b][0], reads=[yt[b][1]], writes=["y_s"])
        P.barrier()
        A.reset()

        ya = [A.tile([128, D], F32, f"ya{i}") for i in range(2)]
        yb = [A.tile([128, D], F32, f"yb{i}") for i in range(2)]
        xo = [A.tile([128, D], F32, f"xo{i}") for i in range(2)]
        for t in range(NT_OWN):
            (a_t, n_a), (b_t, n_b), (o_t, n_o) = ya[t % 2], yb[t % 2], xo[t % 2]
            for k, (dst, n_dst) in enumerate(((a_t, n_a), (b_t, n_b))):
                P.dma('pool', (lambda tt, kk, d_: (lambda e: e.indirect_dma_start(
                    out=d_, out_offset=None, in_=y_s, in_offset=bass.IndirectOffsetOnAxis(ap=idx_all[:, tt, kk:kk + 1], axis=0))))(t, k, dst), reads=["y_s"], writes=[n_dst])
            DMA('sp', o_t, x1_s[t], reads=["x1_s"], writes=[n_o])
            OP('dve', 'scalar_tensor_tensor', reads=[n_a, n_o], writes=[n_o], out=o_t, in0=a_t, scalar=cw_all[:, t, 0:1],
               in1=o_t, op0=ALU.mult, op1=ALU.add)
            OP('dve', 'scalar_tensor_tensor', reads=[n_b, n_o], writes=[n_o], out=o_t, in0=b_t, scalar=cw_all[:, t, 1:2],
               in1=o_t, op0=ALU.mult, op1=ALU.add)
            DMA('sp', out_d[t * 128:(t + 1) * 128, :], o_t, reads=[n_o], writes=["out"])
        P.finish()
        P.emit()
    return nc


def _rot_tables():
    pos = np.arange(SEQ, dtype=np.float32)
    inv = (1.0 / (np.float32(500000.0) ** (np.arange(0, 16, 2, dtype=np.float32) / np.float32(16)))).astype(np.float32)
    ang = (pos[:, None] * inv[None, :]).astype(np.float32)
    return np.cos(ang).astype(np.float32), np.sin(ang).astype(np.float32)


def make_in_maps(inp):
    f = lambda a: np.ascontiguousarray(np.asarray(a, dtype=np.float32))
    x = f(inp["x"])
    w_in = f(inp["w_in"])[0]
    perm = []
    for j in range(4):
        for h in (2 * j, 2 * j + 1):
            perm += list(range(h * 128, (h + 1) * 128))
        for h in (2 * j, 2 * j + 1):
            perm += list(range(1024 + h * 128, 1024 + (h + 1) * 128))
    perm += list(range(2048, 5120))
    w_in_p = np.ascontiguousarray(w_in[:, perm])
    cos, sin = _rot_tables()
    shared = dict(
        g_attn=f(inp["attn_norm_g"]).reshape(1, D), w_in=w_in_p,
        ln_g=f(inp["gmlp_ln_g"]).reshape(1, 1024), ln_b=f(inp["gmlp_ln_b"]).reshape(1, 1024),
        wsT=np.ascontiguousarray(f(inp["gmlp_ws"])[0].transpose(2, 0, 1)),
        bsT=np.ascontiguousarray(f(inp["gmlp_bs"])[0].T),
        qg=np.ascontiguousarray(np.tile(f(inp["q_norm_g"]).reshape(1, 64), (1, 8))),
        kg=np.ascontiguousarray(np.tile(f(inp["k_norm_g"]).reshape(1, 64), (1, 8))),
        lamv=np.concatenate([f(inp[k]).reshape(1, 64) for k in ("lambda_q1", "lambda_q2", "lambda_k1", "lambda_k2")], axis=1),
        subg=f(inp["subln_g"]).reshape(1, 128), w_out=f(inp["w_out"])[0], g_ffn=f(inp["ffn_norm_g"]).reshape(1, D),
        wr=np.ascontiguousarray(np.concatenate([f(inp["w_group"])[0], f(inp["w_router"])[0]], axis=1)),
        br=np.concatenate([f(inp["b_group"]).reshape(1, 4), f(inp["b_router"]).reshape(1, 32)], axis=1),
        w_gate=f(inp["w_gate"])[0], w_up=f(inp["w_up"])[0], w_down=f(inp["w_down"])[0],
    )
    maps = []
    for c in range(8):
        b, s = c // 2, c % 2
        own = slice(s * 2048, (s + 1) * 2048)
        oth = slice((1 - s) * 2048, (2 - s) * 2048)
        xc = np.concatenate([x[b, own], x[b, oth]], axis=0)
        cc = np.concatenate([cos[own], cos[oth]], axis=0).reshape(NT_ALL, 128, 8).transpose(1, 0, 2)
        sc = np.concatenate([sin[own], sin[oth]], axis=0).reshape(NT_ALL, 128, 8).transpose(1, 0, 2)
        m = dict(shared)
        m.update(x=np.ascontiguousarray(xc), cos=np.ascontiguousarray(cc), sin=np.ascontiguousarray(sc))
        maps.append(m)
    return maps


def kernel(**inputs):
    nc = build_program()
    in_maps = make_in_maps(inputs)
    res = run_bass_kernel_spmd(nc, in_maps, core_ids=list(range(8)))
    out = np.empty((4, SEQ, D), np.float32)
    for c in range(8):
        b, s = c // 2, c % 2
        out[b, s * 2048:(s + 1) * 2048] = res.results[c]["out"]
    return out
```
